# Optimizing a Trainium2 kernel written in Bass

```python
import jax, jax.numpy as jnp
from jax import lax
import numpy as np

D_MODEL = 2048
BATCH = 16
SEQ = 256
DEPTH = 2
DEC_BATCH = 4
DEC_SEQ = 1024
PAST_LEN = 256

GRID_W = 64
N_MIXERS = 2
N_A_LAYERS = (DEPTH + 1) // 2
N_B_LAYERS = DEPTH // 2
RWKV_HEAD = 64
RWKV_HEADS = D_MODEL // RWKV_HEAD
DECAY_LORA = 96
AAA_LORA = 96
GATE_LORA = 256
N_SHIFT = 6
POOL_WINDOWS = (2, 4, 8, 16)
N_POOL_GROUPS = len(POOL_WINDOWS)
POOL_GROUP = D_MODEL // N_POOL_GROUPS
N_EXPERTS = 64
TOP_K = 6
N_GROUPS = 8
TOPK_GROUPS = 4
EXPERT_DIM = 512
SHARED_DIM = 512
ROUTED_SCALE = 2.5
ALPHA = (2 * DEPTH) ** 0.25
BETA = (8 * DEPTH) ** -0.25
LN_EPS = 1e-5
GN_EPS = 64e-5

kernel_name = 'bidir_rwkv7_pool_moe_prefix_step'


def layer_norm(x, g, b):
    xf = x.astype(jnp.float32)
    mu = jnp.mean(xf, axis=-1, keepdims=True)
    var = jnp.mean(jnp.square(xf - mu), axis=-1, keepdims=True)
    return ((xf - mu) * lax.rsqrt(var + LN_EPS) * g + b).astype(x.dtype)


def ada_params(cond, w_ada_l, b_ada_l):
    m = jax.nn.silu(cond) @ w_ada_l + b_ada_l
    return jnp.split(m[:, None, :], 6, axis=-1)


def modulate(x, shift, scale):
    return x * (1.0 + scale) + shift


def wkv_scan(S0, r, decay, kk, kka, v, k, reverse):
    def step(S, inp):
        r_t, w_t, kk_t, kka_t, v_t, k_t = inp
        sa = jnp.einsum('bhij,bhj->bhi', S, kk_t)
        S = (S * w_t[:, :, None, :]
             - jnp.einsum('bhi,bhj->bhij', sa, kka_t)
             + jnp.einsum('bhi,bhj->bhij', v_t, k_t))
        return S, jnp.einsum('bhij,bhj->bhi', S, r_t)
    xs = tuple(jnp.swapaxes(t, 0, 1) for t in (r, decay, kk, kka, v, k))
    S, ys = lax.scan(step, S0, xs, reverse=reverse)
    return S, jnp.swapaxes(ys, 0, 1)


def rwkv_mixer(x, S0, mix_prev, mix_next, w_r, w_k, w_v, w_o, w0, w1, w2,
               a0, a1, a2, g1, g2, k_k, k_a, r_k, gn_g, gn_b):
    B, L, D = x.shape
    H, N = RWKV_HEADS, RWKV_HEAD
    f32 = jnp.float32
    d_prev = jnp.pad(x[:, :-1], ((0, 0), (1, 0), (0, 0))) - x
    d_next = jnp.pad(x[:, 1:], ((0, 0), (0, 1), (0, 0))) - x
    xs = x[:, :, None, :] + mix_prev * d_prev[:, :, None, :] + mix_next * d_next[:, :, None, :]
    xr, xw, xk, xv, xa, xg = (xs[:, :, i] for i in range(N_SHIFT))
    to_heads = lambda t: t.reshape(B, L, H, N)
    r = to_heads((xr @ w_r).astype(f32))
    k = (xk @ w_k).astype(f32)
    v = to_heads((xv @ w_v).astype(f32))
    g = jax.nn.sigmoid(xg @ g1) @ g2
    kk = to_heads(k * k_k)
    kk = kk / jnp.maximum(jnp.linalg.norm(kk, axis=-1, keepdims=True), 1e-12)
    if S0 is None:
        S0 = jnp.zeros((B, 2, H, N, N), f32)
    wkv_parts, bonus_parts, finals = [], [], []
    for d in range(2):
        w_log = -jax.nn.softplus(-(w0[d] + jnp.tanh(xw @ w1[d]) @ w2[d]).astype(f32)) - 0.5
        decay = to_heads(jnp.exp(-jnp.exp(w_log)))
        a = jax.nn.sigmoid((a0[d] + (xa @ a1[d]) @ a2[d]).astype(f32))
        k_d = to_heads(k * (1.0 + (a - 1.0) * k_a))
        a = to_heads(a)
        S_fin, y = wkv_scan(S0[:, d].astype(f32), r, decay, kk, kk * a, v, k_d, d == 1)
        wkv_parts.append(y)
        bonus_parts.append(jnp.sum(r * k_d * r_k, axis=-1, keepdims=True) * v)
        finals.append(S_fin)
    y = wkv_parts[0] + wkv_parts[1]
    mu = jnp.mean(y, axis=-1, keepdims=True)
    var = jnp.mean(jnp.square(y - mu), axis=-1, keepdims=True)
    yn = ((y - mu) * lax.rsqrt(var + GN_EPS)).reshape(B, L, D) * gn_g + gn_b
    out = (yn + (bonus_parts[0] + bonus_parts[1]).reshape(B, L, D)) * g
    return out.astype(x.dtype) @ w_o, jnp.stack(finals, axis=1)


def box_sum_1d(x, w, axis):
    L = x.shape[axis]
    cs = jnp.cumsum(x, axis=axis)
    pad = [(0, 0)] * x.ndim
    pad[axis] = (1, 0)
    cs = jnp.pad(cs, pad)
    t = np.arange(L)
    lo = np.clip(t - w // 2, 0, L)
    hi = np.clip(t - w // 2 + w, 0, L)
    s = jnp.take(cs, hi, axis=axis) - jnp.take(cs, lo, axis=axis)
    return s, (hi - lo).astype(np.float32)


def pool_mixer(x, w_pool, pool_scale, grid):
    B, L, D = x.shape
    xf = x.astype(jnp.float32)
    outs = []
    for gi, w in enumerate(POOL_WINDOWS):
        xg = xf[..., gi * POOL_GROUP:(gi + 1) * POOL_GROUP]
        if grid:
            rows = L // GRID_W
            s, cr = box_sum_1d(xg.reshape(B, rows, GRID_W, POOL_GROUP), w, 1)
            s, cc = box_sum_1d(s, w, 2)
            mean = (s / (cr[:, None, None] * cc[None, :, None])).reshape(B, L, POOL_GROUP)
        else:
            s, cnt = box_sum_1d(xg, w, 1)
            mean = s / cnt[:, None]
        outs.append(mean - xg)
    p = jnp.stack(outs, axis=2).astype(x.dtype)
    y = jnp.einsum('blgc,gcd->blgd', p, w_pool).reshape(B, L, D)
    return y * pool_scale


def moe(x, w_router, router_bias, w_gate, w_up, w_down, ws_gate, ws_up, ws_down):
    B, L, D = x.shape
    t = x.reshape(B * L, D)
    T = t.shape[0]
    scores = jax.nn.sigmoid((t @ w_router).astype(jnp.float32))
    biased = scores + router_bias
    grp = biased.reshape(T, N_GROUPS, N_EXPERTS // N_GROUPS)
    grp_score = jnp.sum(lax.top_k(grp, 2)[0], axis=-1)
    _, gidx = lax.top_k(grp_score, TOPK_GROUPS)
    gmask = jnp.sum(jax.nn.one_hot(gidx, N_GROUPS, dtype=jnp.float32), axis=-2)
    emask = jnp.repeat(gmask, N_EXPERTS // N_GROUPS, axis=-1)
    masked = jnp.where(emask > 0, biased, -jnp.inf)
    _, eidx = lax.top_k(masked, TOP_K)
    w_sel = jnp.take_along_axis(scores, eidx, axis=-1)
    w_sel = w_sel / (jnp.sum(w_sel, axis=-1, keepdims=True) + 1e-20) * ROUTED_SCALE
    gates = jnp.sum(jax.nn.one_hot(eidx, N_EXPERTS, dtype=jnp.float32) * w_sel[..., None], axis=-2)
    shared = (jax.nn.silu(t @ ws_gate) * (t @ ws_up)) @ ws_down

    def body(acc, ew):
        wg, wu, wd, ge = ew
        h = jax.nn.silu(t @ wg) * (t @ wu)
        return acc + (h * ge[:, None].astype(h.dtype)) @ wd, None

    out, _ = lax.scan(body, shared, (w_gate, w_up, w_down, gates.T))
    return out.reshape(B, L, D)


def trunk(x, cond, S0_all, grid, w_ada, b_ada, ln_g, ln_b, rwkv_params, pool_params, moe_params):
    states = []
    for l in range(DEPTH):
        sh1, sc1, gt1, sh2, sc2, gt2 = ada_params(cond, w_ada[l], b_ada[l])
        h = modulate(x, sh1, sc1)
        j = l // N_MIXERS
        if l % N_MIXERS == 0:
            S0 = None if S0_all is None else S0_all[:, j]
            mix, S_fin = rwkv_mixer(h, S0, *[p[j] for p in rwkv_params])
            states.append(S_fin)
        else:
            mix = pool_mixer(h, pool_params[0][j], pool_params[1][j], grid)
        x = layer_norm(ALPHA * x + gt1 * mix, ln_g[l, 0], ln_b[l, 0])
        h = modulate(x, sh2, sc2)
        x = layer_norm(ALPHA * x + gt2 * moe(h, *[p[l] for p in moe_params]), ln_g[l, 1], ln_b[l, 1])
    return x, states


def setup_inputs(seed: int = 0) -> dict:
    key = jax.random.key(seed)
    ks = iter(jax.random.split(key, 48))
    nrm = lambda shape, s: jax.random.normal(next(ks), shape, jnp.float32) * s
    D, H, N, E = D_MODEL, RWKV_HEADS, RWKV_HEAD, N_EXPERTS
    return {
        'x_prompt': nrm((BATCH, SEQ, D), 1.0),
        'x_sample': nrm((DEC_BATCH, DEC_SEQ, D), 1.0),
        'c': nrm((DEC_BATCH, D), 1.0),
        'state_rwkv': nrm((DEC_BATCH, N_A_LAYERS, 2, H, N, N), 0.3),
        'c_ctx': nrm((D,), 1.0),
        'w_ada': nrm((DEPTH, D, 6 * D), 0.5 * D ** -0.5),
        'b_ada': nrm((DEPTH, 6 * D), 0.1),
        'ln_g': 1.0 + nrm((DEPTH, 2, D), 0.1),
        'ln_b': nrm((DEPTH, 2, D), 0.01),
        'rw_mix_prev': jax.random.uniform(next(ks), (N_A_LAYERS, N_SHIFT, D), jnp.float32, 0.0, 0.5),
        'rw_mix_next': jax.random.uniform(next(ks), (N_A_LAYERS, N_SHIFT, D), jnp.float32, 0.0, 0.5),
        'rw_w_r': nrm((N_A_LAYERS, D, D), D ** -0.5),
        'rw_w_k': nrm((N_A_LAYERS, D, D), D ** -0.5),
        'rw_w_v': nrm((N_A_LAYERS, D, D), D ** -0.5),
        'rw_w_o': nrm((N_A_LAYERS, D, D), BETA * D ** -0.5),
        'rw_w0': jax.random.uniform(next(ks), (N_A_LAYERS, 2, D), jnp.float32, -6.0, -1.0),
        'rw_w1': nrm((N_A_LAYERS, 2, D, DECAY_LORA), D ** -0.5),
        'rw_w2': nrm((N_A_LAYERS, 2, DECAY_LORA, D), 0.5 * DECAY_LORA ** -0.5),
        'rw_a0': nrm((N_A_LAYERS, 2, D), 0.5),
        'rw_a1': nrm((N_A_LAYERS, 2, D, AAA_LORA), D ** -0.5),
        'rw_a2': nrm((N_A_LAYERS, 2, AAA_LORA, D), 0.5 * AAA_LORA ** -0.5),
        'rw_g1': nrm((N_A_LAYERS, D, GATE_LORA), D ** -0.5),
        'rw_g2': nrm((N_A_LAYERS, GATE_LORA, D), GATE_LORA ** -0.5),
        'rw_k_k': 0.85 + nrm((N_A_LAYERS, D), 0.05),
        'rw_k_a': 1.0 + nrm((N_A_LAYERS, D), 0.05),
        'rw_r_k': nrm((N_A_LAYERS, H, N), 0.1),
        'rw_gn_g': 1.0 + nrm((N_A_LAYERS, D), 0.1),
        'rw_gn_b': nrm((N_A_LAYERS, D), 0.01),
        'pool_w': nrm((N_B_LAYERS, N_POOL_GROUPS, POOL_GROUP, POOL_GROUP), BETA * POOL_GROUP ** -0.5),
        'pool_scale': 1.0 + nrm((N_B_LAYERS, D), 0.1),
        'moe_router': nrm((DEPTH, D, E), D ** -0.5),
        'moe_router_bias': nrm((DEPTH, E), 0.01),
        'moe_w_gate': nrm((DEPTH, E, D, EXPERT_DIM), D ** -0.5),
        'moe_w_up': nrm((DEPTH, E, D, EXPERT_DIM), D ** -0.5),
        'moe_w_down': nrm((DEPTH, E, EXPERT_DIM, D), BETA * EXPERT_DIM ** -0.5),
        'moe_ws_gate': nrm((DEPTH, D, SHARED_DIM), D ** -0.5),
        'moe_ws_up': nrm((DEPTH, D, SHARED_DIM), D ** -0.5),
        'moe_ws_down': nrm((DEPTH, SHARED_DIM, D), BETA * SHARED_DIM ** -0.5),
    }


def reference(x_prompt, x_sample, c, state_rwkv, c_ctx, w_ada, b_ada, ln_g, ln_b,
              rw_mix_prev, rw_mix_next, rw_w_r, rw_w_k, rw_w_v, rw_w_o, rw_w0, rw_w1, rw_w2,
              rw_a0, rw_a1, rw_a2, rw_g1, rw_g2, rw_k_k, rw_k_a, rw_r_k, rw_gn_g, rw_gn_b,
              pool_w, pool_scale, moe_router, moe_router_bias, moe_w_gate, moe_w_up, moe_w_down,
              moe_ws_gate, moe_ws_up, moe_ws_down):
    rwkv_params = (rw_mix_prev, rw_mix_next, rw_w_r, rw_w_k, rw_w_v, rw_w_o, rw_w0, rw_w1, rw_w2,
                   rw_a0, rw_a1, rw_a2, rw_g1, rw_g2, rw_k_k, rw_k_a, rw_r_k, rw_gn_g, rw_gn_b)
    pool_params = (pool_w, pool_scale)
    moe_params = (moe_router, moe_router_bias, moe_w_gate, moe_w_up, moe_w_down,
                  moe_ws_gate, moe_ws_up, moe_ws_down)
    y_prompt, ctx_states = trunk(x_prompt, c_ctx[None, :], None, False, w_ada, b_ada, ln_g, ln_b,
                                 rwkv_params, pool_params, moe_params)
    new_state_rwkv = jnp.stack(ctx_states, axis=1)
    y_sample, _ = trunk(x_sample, c, state_rwkv, True, w_ada, b_ada, ln_g, ln_b,
                        rwkv_params, pool_params, moe_params)
    return (y_prompt, y_sample, new_state_rwkv)
```

```python
import contextlib
import numpy as np
import ml_dtypes
import concourse.bass as bass
import concourse.mybir as mybir
from concourse.bass_utils import run_bass_kernel_spmd

F32 = mybir.dt.float32
BF16 = mybir.dt.bfloat16
AF = mybir.ActivationFunctionType
ALU = mybir.AluOpType
AX = mybir.AxisListType

D = 2048
NT = 1024
NQ = 16
NE = 64
ALPHA = (2 * 2) ** 0.25
LN_EPS = 1e-5
GN_EPS = 64e-5
CH = 64
NCH = NT // CH


class T:
    __slots__ = ("h", "w", "r", "name")

    def __init__(self, h, name):
        self.h = h
        self.name = name
        self.w = None
        self.r = []

    def __getitem__(self, idx):
        return self.h[idx]


class TV(T):
    __slots__ = ("ap",)

    def __init__(self, ap, name):
        T.__init__(self, None, name)
        self.ap = ap

    def __getitem__(self, idx):
        return self.ap[idx]


class KB:
    ENG = ("pe", "act", "dve", "pool", "sp")

    def __init__(self, nc, n_dma_sems=32):
        self.nc = nc
        self.es = contextlib.ExitStack()
        self.e = {"pe": nc.tensor, "act": nc.scalar, "dve": nc.vector,
                  "pool": nc.gpsimd, "sp": nc.sync}
        self.sem = {k: self.es.enter_context(nc.semaphore("s_" + k)) for k in self.ENG}
        self.cnt = {k: 0 for k in self.ENG}
        self.seen = {k: {} for k in self.ENG}
        self.dsem = [self.es.enter_context(nc.semaphore("d%d" % i)) for i in range(n_dma_sems)]
        self.dval = [0] * n_dma_sems
        half = n_dma_sems // 2
        self.dpool = {"sp": list(range(0, half)), "act": list(range(0, half)), "pool": list(range(half, n_dma_sems))}
        self.dnext = {"sp": 0, "act": 0, "pool": 0}
        self.nalloc = 0
        self.ninst = 0

    def sb(self, shape, dt=F32, name=None, stack=None):
        self.nalloc += 1
        name = (name or "t") + "_%d" % self.nalloc
        h = (stack or self.es).enter_context(self.nc.sbuf_tensor(name, list(shape), dt))
        return T(h, name)

    def ps(self, shape, dt=F32, name=None, stack=None):
        self.nalloc += 1
        name = (name or "p") + "_%d" % self.nalloc
        h = (stack or self.es).enter_context(self.nc.psum_tensor(name, list(shape), dt))
        return T(h, name)

    def view(self, t, name=None):
        return T(t.h, name or t.name + "_v")

    def _need(self, eng, dep):
        if dep is None:
            return
        if dep[0] == "e":
            _, de, val = dep
            key = de
            sem = self.sem[de]
        else:
            _, si, val = dep
            key = "d%d" % si
            sem = self.dsem[si]
        if self.seen[eng].get(key, 0) >= val:
            return
        self.e[eng].wait_ge(sem, val)
        self.ninst += 1
        self.seen[eng][key] = val

    def _deps(self, eng, reads, writes):
        for t in reads:
            if eng == "pe" and t.w is not None and t.w[0] == "e" and t.w[1] == "pe":
                continue
            self._need(eng, t.w)
        for t in writes:
            same_pe = eng == "pe"
            if not (same_pe and t.w is not None and t.w[0] == "e" and t.w[1] == "pe"):
                self._need(eng, t.w)
            for d in t.r:
                if same_pe and d[0] == "e" and d[1] == "pe":
                    continue
                self._need(eng, d)

    def op(self, eng, fn, reads=(), writes=()):
        self._deps(eng, reads, writes)
        ins = fn(self.e[eng])
        self.cnt[eng] += 1
        ins.then_inc(self.sem[eng], 1)
        self.ninst += 1
        me = ("e", eng, self.cnt[eng])
        for t in reads:
            t.r.append(me)
            if len(t.r) > 48:
                best = {}
                for d in t.r:
                    kk = (d[0], d[1])
                    if kk not in best or best[kk][2] < d[2]:
                        best[kk] = d
                t.r = list(best.values())
        for t in writes:
            t.w = me
            t.r = []
        return ins

    def dma(self, eng, out, in_, reads=(), writes=(), **kw):
        self._deps(eng, reads, writes)
        pool_ = self.dpool[eng]
        qk = "sp" if eng == "act" else eng
        si = pool_[self.dnext[qk] % len(pool_)]
        self.dnext[qk] += 1
        self._need(eng, ("d", si, self.dval[si]))
        ins = self.e[eng].dma_start(out=out, in_=in_, **kw)
        self.dval[si] += 16
        ins.then_inc(self.dsem[si], 16)
        self.ninst += 1
        me = ("d", si, self.dval[si])
        for t in reads:
            t.r.append(me)
        for t in writes:
            t.w = me
            t.r = []
        return me

    def barrier(self):
        for eng in self.ENG:
            for other in self.ENG:
                if other != eng and self.cnt[other] > 0:
                    self._need(eng, ("e", other, self.cnt[other]))
            for si in range(len(self.dsem)):
                if self.dval[si] > 0:
                    self._need(eng, ("d", si, self.dval[si]))

    def finish(self, eng="sp"):
        for si in range(len(self.dsem)):
            if self.dval[si] > 0:
                self._need(eng, ("d", si, self.dval[si]))
        for other in self.ENG:
            if other != eng and self.cnt[other] > 0:
                self._need(eng, ("e", other, self.cnt[other]))

    def close(self):
        self.es.close()

def _col_layout():
    off = {}
    n = 0
    def add(name, w=16):
        nonlocal n
        off[name] = n
        n += w
    for l in range(2):
        add("b_ada%d" % l, 96)
    for l in range(2):
        for s in range(2):
            add("ln_g%d%d" % (l, s)); add("ln_b%d%d" % (l, s))
    for i in range(6):
        add("mp%d" % i); add("mn%d" % i)
    for d in range(2):
        add("w0%d" % d); add("a0%d" % d)
    for nm in ("k_k", "k_a", "r_k", "gn_g", "gn_b", "pool_scale"):
        add(nm)
    return off, n

COLOFF, NCOL = _col_layout()


def _colvec(v):
    return np.ascontiguousarray(np.asarray(v, np.float32).reshape(-1, 128).T)


def build_program(dbg=None):
    nc = bass.Bass("TRN2", target_bir_lowering=False)
    in_names = []
    def dt_in(name, shape, dt=F32):
        in_names.append(name)
        return nc.dram_tensor(name, list(shape), dt, kind="ExternalInput").ap()
    need_moe = dbg not in ("ada", "p2", "rwkv", "mix", "scan1")
    x_d = dt_in("x", [NT, D])
    cond_d = dt_in("cond", [128, 16])
    s0_d = dt_in("s0", [2, 32, 64, 64])
    keep_d = dt_in("keepc", [128, 2, NCH])
    tmask_d = dt_in("tmask", [128, 2, NT])
    poolMT_d = dt_in("poolMT", [4, NT, NT], BF16)
    poolic_d = dt_in("poolic", [128, 4, NT])
    cols_d = dt_in("cols", [128, NCOL])
    rbias_d = dt_in("rbias", [128, 2, NE])
    w_ada_d = dt_in("w_ada", [2, D, 6 * D])
    w_r_d = dt_in("rw_w_r", [D, D]); w_k_d = dt_in("rw_w_k", [D, D])
    w_v_d = dt_in("rw_w_v", [D, D]); w_o_d = dt_in("rw_w_o", [D, D])
    w1_d = dt_in("rw_w1", [2, D, 96]); w2_d = dt_in("rw_w2", [2, 96, D])
    a1_d = dt_in("rw_a1", [2, D, 96]); a2_d = dt_in("rw_a2", [2, 96, D])
    g1_d = dt_in("rw_g1", [D, 256]); g2_d = dt_in("rw_g2", [256, D])
    pool_w_d = dt_in("pool_w", [4, 512, 512])
    router_d = dt_in("moe_router", [2, D, NE])
    if need_moe:
        wg_d = dt_in("moe_w_gate", [2, NE, D, 512]); wu_d = dt_in("moe_w_up", [2, NE, D, 512])
        wd_d = dt_in("moe_w_down", [2, NE, 512, D])
    wsg_d = dt_in("moe_ws_gate", [2, D, 512]); wsu_d = dt_in("moe_ws_up", [2, D, 512])
    wsd_d = dt_in("moe_ws_down", [2, 512, D])
    y_d = nc.dram_tensor("y", [NT, D], F32, kind="ExternalOutput").ap()
    ns_d = nc.dram_tensor("ns", [4, 2, 32, 64, 64], F32, kind="ExternalOutput").ap()
    rkv_d = nc.dram_tensor("rkv_scratch", [3, NQ, 128, NT], F32, kind="Internal").ap()
    dbg_d = None
    if dbg is not None:
        dbg_d = nc.dram_tensor("dbg", [NQ, 128, NT], F32, kind="ExternalOutput").ap()

    k = KB(nc)
    V = lambda eng, fn, r=(), w=(): k.op(eng, fn, reads=r, writes=w)

    ident = k.sb([128, 128], F32, "ident")
    V("pool", lambda e: e.memset(ident[:], 0.0), w=[ident])
    V("pool", lambda e: e.affine_select(ident[:], ident[:], pattern=[[-1, 128]], compare_op=ALU.not_equal,
                                        fill=1.0, base=0, channel_multiplier=1), r=[ident], w=[ident])
    identb = k.sb([128, 128], BF16, "identb")
    V("dve", lambda e: e.tensor_copy(identb[:], ident[:]), r=[ident], w=[identb])
    ones_ln = k.sb([128, 128], F32, "ones_ln")
    V("pool", lambda e: e.memset(ones_ln[:], 1.0 / D), w=[ones_ln])
    blk1 = k.sb([128, 128], F32, "blk1")
    V("pool", lambda e: e.memset(blk1[:], 0.0), w=[blk1])
    V("pool", lambda e: e.memset(blk1[0:64, 0:64], 1.0), w=[blk1])
    V("pool", lambda e: e.memset(blk1[64:128, 64:128], 1.0), w=[blk1])
    blkm = k.sb([128, 128], F32, "blkm")
    V("dve", lambda e: e.tensor_scalar(blkm[:], blk1[:], 1.0 / 64, None, op0=ALU.mult), r=[blk1], w=[blkm])

    cols = k.sb([128, NCOL], F32, "cols")
    k.dma("sp", cols[:], cols_d, writes=[cols])
    C = lambda name, q=0, w=1: cols[:, COLOFF[name] + q: COLOFF[name] + q + w]

    def tri_mask(name, cmp_keep, zero_block, sgn=1):
        m = k.sb([128, 128], F32, name)
        V("pool", lambda e: e.memset(m[:], 1.0), w=[m])
        V("pool", lambda e: e.affine_select(m[:], m[:], pattern=[[sgn, 128]], compare_op=cmp_keep,
                                            fill=0.0, base=0, channel_multiplier=-sgn), r=[m], w=[m])
        if zero_block == "ur":
            V("pool", lambda e: e.memset(m[0:64, 64:128], 0.0), w=[m])
        else:
            V("pool", lambda e: e.memset(m[64:128, 0:64], 0.0), w=[m])
        return m
    UT_s = tri_mask("UTs", ALU.is_gt, "ur")
    UT_i = tri_mask("UTi", ALU.is_ge, "ur")
    LT_s = tri_mask("LTs", ALU.is_gt, "ll", -1)
    LT_i = tri_mask("LTi", ALU.is_ge, "ll", -1)
    MK1, MK2, MKA = [], [], []
    for d in range(2):
        sT, iT, sA = (UT_s, UT_i, LT_s) if d == 0 else (LT_s, LT_i, UT_s)
        m1 = k.sb([128, 256], F32, "MK1"); m2 = k.sb([128, 256], F32, "MK2"); ma = k.sb([128, 128], F32, "MKA")
        V("dve", lambda e: e.tensor_scalar(m1[:, 0:128], sT[:], -1.0, None, op0=ALU.mult), r=[sT], w=[m1])
        V("dve", lambda e: e.tensor_copy(m1[:, 128:256], iT[:]), r=[iT], w=[m1])
        V("dve", lambda e: e.tensor_copy(m2[:, 0:128], sT[:]), r=[sT], w=[m2])
        V("dve", lambda e: e.tensor_copy(m2[:, 128:256], iT[:]), r=[iT], w=[m2])
        V("dve", lambda e: e.tensor_scalar(ma[:], sA[:], -1.0, None, op0=ALU.mult), r=[sA], w=[ma])
        MK1.append(m1); MK2.append(m2); MKA.append(ma)
    bmask = k.sb([128, 2, CH], F32, "bmask")
    V("pool", lambda e: e.memset(bmask[:], 0.0), w=[bmask])
    V("pool", lambda e: e.memset(bmask[0:64, 0, :], 1.0), w=[bmask])
    V("pool", lambda e: e.memset(bmask[64:128, 1, :], 1.0), w=[bmask])
    cmask = k.sb([128, NCH, CH], F32, "cmask")
    V("pool", lambda e: e.memset(cmask[:], 1.0), w=[cmask])
    V("pool", lambda e: e.memset(cmask[:, :, 0:1], 0.0), w=[cmask])
    keepc = k.sb([128, 2, NCH], F32, "keepc")
    k.dma("sp", keepc[:], keep_d, writes=[keepc])
    tmask = k.sb([128, 2, NT], F32, "tmask")
    k.dma("sp", tmask[:], tmask_d, writes=[tmask])

    ada = k.sb([128, 2, 96], F32, "ada")
    opsc = k.sb([128, 2, 2, 16], F32, "opsc")
    with contextlib.ExitStack() as st:
        cond = k.sb([128, 16], F32, "cond", st)
        k.dma("sp", cond[:], cond_d, writes=[cond])
        scond = k.sb([128, 16], BF16, "scond", st)
        V("act", lambda e: e.activation(scond[:], cond[:], AF.Silu), r=[cond], w=[scond])
        wbuf = [k.sb([128, 16, 512], BF16, "adaw", st) for _ in range(3)]
        adap = k.ps([128, 2, 96], F32, "adap", st)
        n = 0
        for l in range(2):
            wv = w_ada_d[l].rearrange("(c p) n -> p c n", p=128)
            for pc in range(24):
                wb = wbuf[n % 3]; n += 1
                k.dma("pool", wb[:], wv[:, :, pc * 512:(pc + 1) * 512], writes=[wb])
                for j in range(4):
                    col = pc * 4 + j
                    for c in range(16):
                        V("pe", lambda e, wb=wb, j=j, c=c, col=col, l=l: e.matmul(
                            adap[:, l, col:col + 1], wb[:, c, j * 128:(j + 1) * 128], scond[:, c:c + 1],
                            start=(c == 0), stop=(c == 15)), r=[wb, scond], w=[adap])
            V("dve", lambda e, l=l: e.tensor_tensor(ada[:, l, :], adap[:, l, :], C("b_ada%d" % l, 0, 96), op=ALU.add),
              r=[adap, cols], w=[ada])
            for s in range(2):
                V("dve", lambda e, l=l, s=s: e.tensor_scalar(opsc[:, l, s, :], ada[:, l, (3 * s + 1) * 16:(3 * s + 2) * 16],
                                                          1.0, None, op0=ALU.add), r=[ada], w=[opsc])
        k.barrier()
    ADA = lambda l, part, q: ada[:, l, part * 16 + q: part * 16 + q + 1]
    OPSC = lambda l, s, q: opsc[:, l, s, q:q + 1]

    xT_all = k.sb([128, NQ, NT], F32, "xT")
    xT = [k.view(xT_all, "xT%d" % q) for q in range(NQ)]
    XT = lambda q: xT_all[:, q, :]
    bfA = k.sb([128, NQ, NT], BF16, "bfA")

    def load_xT(st):
        xtok = [k.sb([128, D], F32, "xtok", st) for _ in range(2)]
        tp = [k.ps([128, 4, 128], F32, "tp", st) for _ in range(2)]
        n = 0
        for tt in range(8):
            xb = xtok[tt % 2]
            k.dma("sp", xb[:], x_d[tt * 128:(tt + 1) * 128, :], writes=[xb])
            for qg in range(4):
                p = tp[n % 2]; n += 1
                for j in range(4):
                    q = qg * 4 + j
                    V("pe", lambda e, p=p, j=j, q=q, xb=xb: e.transpose(p[:, j, :], xb[:, q * 128:(q + 1) * 128], ident[:]),
                      r=[xb, ident], w=[p])
                eng = "act" if (n % 2) else "dve"
                if eng == "act":
                    V("act", lambda e, p=p, qg=qg, tt=tt: e.activation(xT_all[:, qg * 4:qg * 4 + 4, tt * 128:(tt + 1) * 128], p[:], AF.Copy),
                      r=[p], w=[xT[qg * 4 + j] for j in range(4)])
                else:
                    V("dve", lambda e, p=p, qg=qg, tt=tt: e.tensor_copy(xT_all[:, qg * 4:qg * 4 + 4, tt * 128:(tt + 1) * 128], p[:]),
                      r=[p], w=[xT[qg * 4 + j] for j in range(4)])

    def layer_norm(l, s, st):
        sq = [k.sb([128, 512], F32, "lnsq", st) for _ in range(2)]
        mean = k.sb([128, 512], F32, "lnmean", st)
        rstd = k.sb([128, 512], F32, "lnrstd", st)
        pm = k.ps([128, 512], F32, "lnpm", st)
        p2 = k.ps([128, 512], F32, "lnp2", st)
        for half in range(2):
            hs = slice(half * 512, (half + 1) * 512)
            for q in range(NQ):
                V("pe", lambda e, q=q: e.matmul(pm[:], ones_ln[:], xT_all[:, q, hs], start=(q == 0), stop=(q == NQ - 1)),
                  r=[ones_ln, xT[q]], w=[pm])
            for q in range(NQ):
                s_ = sq[q % 2]
                V("act", lambda e, q=q, s_=s_: e.activation(s_[:], xT_all[:, q, hs], AF.Square), r=[xT[q]], w=[s_])
                V("pe", lambda e, q=q, s_=s_: e.matmul(p2[:], ones_ln[:], s_[:], start=(q == 0), stop=(q == NQ - 1)),
                  r=[ones_ln, s_], w=[p2])
            V("act", lambda e: e.activation(mean[:], pm[:], AF.Copy), r=[pm], w=[mean])
            V("dve", lambda e: e.tensor_tensor(rstd[:], mean[:], mean[:], op=ALU.mult), r=[mean], w=[rstd])
            V("dve", lambda e: e.tensor_tensor(rstd[:], p2[:], rstd[:], op=ALU.subtract), r=[p2, rstd], w=[rstd])
            V("dve", lambda e: e.tensor_scalar(rstd[:], rstd[:], 0.0, LN_EPS, op0=ALU.max, op1=ALU.add), r=[rstd], w=[rstd])
            V("act", lambda e: e.activation(rstd[:], rstd[:], AF.Ln), r=[rstd], w=[rstd])
            V("act", lambda e: e.activation(rstd[:], rstd[:], AF.Exp, scale=-0.5), r=[rstd], w=[rstd])
            for q in range(NQ):
                eng = "dve" if q % 2 == 0 else "pool"
                V(eng, lambda e, q=q: e.tensor_tensor(xT_all[:, q, hs], xT_all[:, q, hs], mean[:], op=ALU.subtract),
                  r=[xT[q], mean], w=[xT[q]])
                V(eng, lambda e, q=q: e.tensor_tensor(xT_all[:, q, hs], xT_all[:, q, hs], rstd[:], op=ALU.mult),
                  r=[xT[q], rstd], w=[xT[q]])
                V(eng, lambda e, q=q: e.tensor_scalar(xT_all[:, q, hs], xT_all[:, q, hs], C("ln_g%d%d" % (l, s), q),
                                                      C("ln_b%d%d" % (l, s), q), op0=ALU.mult, op1=ALU.add),
                  r=[xT[q], cols], w=[xT[q]])

    def moe(l, st0):
        st = contextlib.ExitStack()
        h2_all = bfA
        h2 = [k.view(h2_all, "h2_%d" % q) for q in range(NQ)]
        gT = k.sb([64, NT], F32, "gT", st)
        with contextlib.ExitStack() as s1:
            wr = k.sb([128, NQ, NE], F32, "wr", s1)
            k.dma("sp", wr[:], router_d[l].rearrange("(c p) n -> p c n", p=128), writes=[wr])
            rb = k.sb([128, NE], F32, "rb", s1)
            k.dma("sp", rb[:], rbias_d[:, l, :], writes=[rb])
            hf = [k.sb([128, NT], F32, "h2f", s1) for _ in range(2)]
            s1a = contextlib.ExitStack()
            lgs = [k.ps([128, 512], F32, "lg", s1a) for _ in range(8)]
            for q in range(NQ):
                h_ = hf[q % 2]
                V("dve", lambda e: e.tensor_scalar(h_[:], XT(q), OPSC(l, 1, q), ADA(l, 3, q), op0=ALU.mult, op1=ALU.add),
                  r=[xT[q], opsc, ada], w=[h_])
                V("act", lambda e: e.activation(h2_all[:, q, :], h_[:], AF.Copy), r=[h_], w=[h2[q]])
                for tt in range(8):
                    V("pe", lambda e: e.matmul(lgs[tt][:, 0:NE], h_[:, tt * 128:(tt + 1) * 128], wr[:, q, :],
                                               start=(q == 0), stop=(q == NQ - 1)),
                      r=[h_, wr], w=[lgs[tt]])
                V("pool", lambda e: e.tensor_scalar(XT(q), XT(q), ALPHA, None, op0=ALU.mult), r=[xT[q]], w=[xT[q]])
            sc = k.sb([128, 8, NE], F32, "sc", s1)
            for tt in range(8):
                V("act", lambda e: e.activation(sc[:, tt, :], lgs[tt][:, 0:NE], AF.Sigmoid), r=[lgs[tt]], w=[sc])
            k.barrier()
            s1a.close()
            bi = k.sb([128, 8, NE], F32, "bi", s1)
            V("dve", lambda e: e.tensor_tensor(bi[:], sc[:], rb[:].unsqueeze(1).broadcast_to([128, 8, NE]), op=ALU.add),
              r=[sc, rb], w=[bi])
            m1 = k.sb([128, 64], F32, "m1", s1); m2 = k.sb([128, 64], F32, "m2", s1)
            tmp = k.sb([128, 8, NE], F32, "rtmp", s1)
            bi3 = bi[:].rearrange("p a (g e) -> p (a g) e", e=8)
            tmp3 = tmp[:].rearrange("p a (g e) -> p (a g) e", e=8)
            V("dve", lambda e: e.tensor_reduce(m1[:], bi3, axis=AX.X, op=ALU.max), r=[bi], w=[m1])
            V("dve", lambda e: e.tensor_tensor(tmp3, bi3, m1[:].unsqueeze(2).broadcast_to([128, 64, 8]), op=ALU.is_equal),
              r=[bi, m1], w=[tmp])
            V("dve", lambda e: e.scalar_tensor_tensor(tmp[:], tmp[:], -1e30, bi[:], op0=ALU.mult, op1=ALU.add), r=[tmp, bi], w=[tmp])
            V("dve", lambda e: e.tensor_reduce(m2[:], tmp3, axis=AX.X, op=ALU.max), r=[tmp], w=[m2])
            V("dve", lambda e: e.tensor_tensor(m1[:], m1[:], m2[:], op=ALU.add), r=[m1, m2], w=[m1])
            srt = k.sb([128, 8, 8], F32, "srt", s1)
            for tt in range(8):
                V("dve", lambda e: e.max(srt[:, tt, :], m1[:, tt * 8:(tt + 1) * 8]), r=[m1], w=[srt])
            gm = k.sb([128, 64], F32, "gm", s1)
            V("dve", lambda e: e.tensor_tensor(gm[:].rearrange("p (a g) -> p a g", g=8), m1[:].rearrange("p (a g) -> p a g", g=8),
                                               srt[:, :, 3:4].broadcast_to([128, 8, 8]), op=ALU.is_ge), r=[m1, srt], w=[gm])
            V("dve", lambda e: e.tensor_scalar(gm[:], gm[:], -1.0, 1e30, op0=ALU.add, op1=ALU.mult), r=[gm], w=[gm])
            V("dve", lambda e: e.tensor_tensor(tmp3, bi3, gm[:].unsqueeze(2).broadcast_to([128, 64, 8]), op=ALU.add),
              r=[bi, gm], w=[tmp])
            for tt in range(8):
                V("dve", lambda e: e.max(srt[:, tt, :], tmp[:, tt, :]), r=[tmp], w=[srt])
            sel = k.sb([128, 8, NE], F32, "sel", s1)
            V("dve", lambda e: e.tensor_tensor(sel[:], tmp[:], srt[:, :, 5:6].broadcast_to([128, 8, NE]), op=ALU.is_ge),
              r=[tmp, srt], w=[sel])
            V("dve", lambda e: e.tensor_tensor(sel[:], sel[:], sc[:], op=ALU.mult), r=[sel, sc], w=[sel])
            den = k.sb([128, 8], F32, "den", s1)
            V("dve", lambda e: e.tensor_reduce(den[:], sel[:], axis=AX.X, op=ALU.add), r=[sel], w=[den])
            V("dve", lambda e: e.tensor_scalar(den[:], den[:], 1e-20, None, op0=ALU.add), r=[den], w=[den])
            V("dve", lambda e: e.reciprocal(den[:], den[:]), r=[den], w=[den])
            V("dve", lambda e: e.tensor_scalar(den[:], den[:], 2.5, None, op0=ALU.mult), r=[den], w=[den])
            V("dve", lambda e: e.tensor_tensor(sel[:], sel[:], den[:].unsqueeze(2).broadcast_to([128, 8, NE]), op=ALU.mult),
              r=[sel, den], w=[sel])
            gtp = k.ps([64, NT], F32, "gtp", s1)
            for tt in range(8):
                V("pe", lambda e: e.transpose(gtp[:, tt * 128:(tt + 1) * 128], sel[:, tt, :], ident[:]), r=[sel, ident], w=[gtp])
            V("act", lambda e: e.activation(gT[:], gtp[:], AF.Copy), r=[gtp], w=[gT])
            k.barrier()
        gu = [k.sb([128, 16, 256], BF16, "gu", st) for _ in range(4)]
        dd = [k.sb([128, 2, D], BF16, "dd", st) for _ in range(2)]
        sgb = [k.sb([128, 512], F32, "sg", st) for _ in range(2)]
        t1b = [k.sb([128, 512], F32, "t1", st) for _ in range(2)]
        hact_all = [k.sb([128, 4, NT], BF16, "hact", st) for _ in range(2)]
        gbc = [k.sb([128, NT], F32, "gbc", st) for _ in range(2)]
        g_ps = [k.ps([128, 512], F32, "gps", st) for _ in range(2)]
        u_ps = [k.ps([128, 512], F32, "ups", st) for _ in range(2)]
        d_ps = [k.ps([128, 512], F32, "dps", st) for _ in range(2)]
        b_ps = [k.ps([128, 512], F32, "bps", st) for _ in range(1)]
        cnt = {"g": 0, "d": 0}
        for ei in range(-1, NE):
            if ei < 0:
                wgv = wsg_d[l].rearrange("(c p) n -> p c n", p=128)
                wuv = wsu_d[l].rearrange("(c p) n -> p c n", p=128)
                wdv = wsd_d[l].rearrange("(c p) n -> p c n", p=128)
            else:
                wgv = wg_d[l, ei].rearrange("(c p) n -> p c n", p=128)
                wuv = wu_d[l, ei].rearrange("(c p) n -> p c n", p=128)
                wdv = wd_d[l, ei].rearrange("(c p) n -> p c n", p=128)
            for hh in range(2):
                k.dma("pool", gu[2 * hh][:], wgv[:, :, hh * 256:(hh + 1) * 256], writes=[gu[2 * hh]])
                k.dma("pool", gu[2 * hh + 1][:], wuv[:, :, hh * 256:(hh + 1) * 256], writes=[gu[2 * hh + 1]])
            for hh in range(2):
                k.dma("pool", dd[hh][:], wdv[:, 2 * hh:2 * hh + 2, :], writes=[dd[hh]])
            ha = hact_all[(ei + 1) % 2]
            gb = gbc[(ei + 1) % 2]
            if ei >= 0:
                for half in range(2):
                    bp = b_ps[0]
                    V("pe", lambda e: e.matmul(bp[:], ident[0:64, ei:ei + 1].broadcast_to([64, 128]),
                                               gT[:, half * 512:(half + 1) * 512], start=True, stop=True), r=[ident, gT], w=[bp])
                    V("act", lambda e: e.activation(gb[:, half * 512:(half + 1) * 512], bp[:], AF.Copy), r=[bp], w=[gb])
            for j in range(4):
                wgp = gu[2 * (j // 2)]; wup = gu[2 * (j // 2) + 1]; jc = (j % 2) * 128
                for half in range(2):
                    hs = slice(half * 512, (half + 1) * 512)
                    gp = g_ps[cnt["g"] % 2]; up = u_ps[cnt["g"] % 2]
                    sg = sgb[cnt["g"] % 2]; t1 = t1b[cnt["g"] % 2]; cnt["g"] += 1
                    for c in range(NQ):
                        V("pe", lambda e: e.matmul(gp[:], wgp[:, c, jc:jc + 128], h2_all[:, c, hs], start=(c == 0), stop=(c == NQ - 1)),
                          r=[wgp, h2[c]], w=[gp])
                    for c in range(NQ):
                        V("pe", lambda e: e.matmul(up[:], wup[:, c, jc:jc + 128], h2_all[:, c, hs], start=(c == 0), stop=(c == NQ - 1)),
                          r=[wup, h2[c]], w=[up])
                    V("act", lambda e: e.activation(sg[:], gp[:], AF.Silu), r=[gp], w=[sg])
                    if ei >= 0:
                        V("dve", lambda e: e.tensor_tensor(t1[:], up[:], sg[:], op=ALU.mult), r=[up, sg], w=[t1])
                        V("dve", lambda e: e.tensor_tensor(ha[:, j, hs], t1[:], gb[:, hs], op=ALU.mult), r=[t1, gb], w=[ha])
                    else:
                        V("dve", lambda e: e.tensor_tensor(ha[:, j, hs], up[:], sg[:], op=ALU.mult), r=[up, sg], w=[ha])
            for f in range(NQ):
                for half in range(2):
                    hs = slice(half * 512, (half + 1) * 512)
                    dp = d_ps[cnt["d"] % 2]; cnt["d"] += 1
                    for j in range(4):
                        V("pe", lambda e: e.matmul(dp[:], dd[j // 2][:, j % 2, f * 128:(f + 1) * 128], ha[:, j, hs],
                                                   start=(j == 0), stop=(j == 3)), r=[dd[j // 2], ha], w=[dp])
                    V("dve", lambda e: e.scalar_tensor_tensor(xT_all[:, f, hs], dp[:], ADA(l, 5, f), xT_all[:, f, hs],
                                                              op0=ALU.mult, op1=ALU.add), r=[dp, ada, xT[f]], w=[xT[f]])
        k.barrier()
        st.close()

    def rwkv_layer(l=0):
        st = contextlib.ExitStack()
        with contextlib.ExitStack() as s0_:
            load_xT(s0_)
            k.barrier()
        tw = [k.sb([96, NT], BF16, "tw", st) for _ in range(2)]
        ta = [k.sb([96, NT], BF16, "ta", st) for _ in range(2)]
        sgl = k.sb([128, 2, NT], BF16, "sgl", st)
        negw0 = k.sb([128, 2, 16], F32, "negw0", st)
        omka = k.sb([128, 16], F32, "omka", st)
        for d in range(2):
            V("dve", lambda e: e.tensor_scalar(negw0[:, d, :], C("w0%d" % d, 0, 16), -1.0, None, op0=ALU.mult), r=[cols], w=[negw0])
        V("dve", lambda e: e.tensor_scalar(omka[:], C("k_a", 0, 16), -1.0, 1.0, op0=ALU.mult, op1=ALU.add), r=[cols], w=[omka])

        def mixes(q, idxs, outs, hp, dp, dn, tmpx):
            V("dve", lambda e: e.tensor_scalar(hp[:, 1:NT + 1], XT(q), OPSC(l, 0, q), ADA(l, 0, q), op0=ALU.mult, op1=ALU.add),
              r=[xT[q], opsc, ada], w=[hp])
            V("pool", lambda e: e.tensor_tensor(dp[:], hp[:, 0:NT], tmask[:, 0, :], op=ALU.mult), r=[hp, tmask], w=[dp])
            V("pool", lambda e: e.tensor_tensor(dp[:], dp[:], hp[:, 1:NT + 1], op=ALU.subtract), r=[dp, hp], w=[dp])
            V("dve", lambda e: e.tensor_tensor(dn[:], hp[:, 2:NT + 2], tmask[:, 1, :], op=ALU.mult), r=[hp, tmask], w=[dn])
            V("dve", lambda e: e.tensor_tensor(dn[:], dn[:], hp[:, 1:NT + 1], op=ALU.subtract), r=[dn, hp], w=[dn])
            for n_, (i, (ot, oap)) in enumerate(zip(idxs, outs)):
                eng = "dve"
                V(eng, lambda e: e.scalar_tensor_tensor(tmpx[:], dp[:], C("mp%d" % i, q), hp[:, 1:NT + 1], op0=ALU.mult, op1=ALU.add),
                  r=[dp, hp, cols], w=[tmpx])
                V(eng, lambda e: e.scalar_tensor_tensor(oap, dn[:], C("mn%d" % i, q), tmpx[:], op0=ALU.mult, op1=ALU.add),
                  r=[dn, tmpx, cols], w=[ot])

        with contextlib.ExitStack() as s1:
            hp = k.sb([128, NT + 2], F32, "hp", s1)
            V("pool", lambda e: e.memset(hp[:], 0.0), w=[hp])
            dp = k.sb([128, NT], F32, "dp", s1); dn = k.sb([128, NT], F32, "dn", s1); tmpx = k.sb([128, NT], F32, "tmpx", s1)
            if dbg == "mix":
                xo = k.sb([128, NT], BF16, "xo", s1)
                mixes(3, [0], [(xo, xo[:])], hp, dp, dn, tmpx)
                k.dma("sp", dbg_d[0], hp[:, 1:NT + 1], reads=[hp])
                k.dma("sp", dbg_d[1], dp[:], reads=[dp])
                k.dma("sp", dbg_d[2], dn[:], reads=[dn])
                k.dma("sp", dbg_d[3], tmpx[:], reads=[tmpx])
                xf = k.sb([128, NT], F32, "xf", s1)
                V("dve", lambda e: e.tensor_copy(xf[:], xo[:]), r=[xo], w=[xf])
                k.dma("sp", dbg_d[4], xf[:], reads=[xf])
                k.dma("sp", dbg_d[5], tmask[:, 0, :], reads=[tmask])
                k.dma("sp", dbg_d[6], tmask[:, 1, :], reads=[tmask])
                k.barrier()
                return st, None, None, None, None, None
            with contextlib.ExitStack() as s2:
                w1s = k.sb([128, NQ, 2, 96], BF16, "w1s", s2); a1s = k.sb([128, NQ, 2, 96], BF16, "a1s", s2)
                g1s = k.sb([128, NQ, 256], BF16, "g1s", s2)
                for d in range(2):
                    k.dma("pool", w1s[:, :, d, :], w1_d[d].rearrange("(c p) n -> p c n", p=128), writes=[w1s])
                    k.dma("pool", a1s[:, :, d, :], a1_d[d].rearrange("(c p) n -> p c n", p=128), writes=[a1s])
                k.dma("pool", g1s[:], g1_d.rearrange("(c p) n -> p c n", p=128), writes=[g1s])
                xs2 = [k.sb([128, NT], BF16, "xs2", s2) for _ in range(4)]
                pp = [k.ps([128, 512], F32, "lp", s2) for _ in range(8)]
                for q in range(NQ):
                    xw = xs2[(q % 2) * 2]; xg = xs2[(q % 2) * 2 + 1]
                    mixes(q, [1, 5], [(xw, xw[:]), (xg, xg[:])], hp, dp, dn, tmpx)
                    for d in range(2):
                        for half in range(2):
                            V("pe", lambda e: e.matmul(pp[d * 2 + half][0:96, :], w1s[:, q, d, :], xw[:, half * 512:(half + 1) * 512],
                                                       start=(q == 0), stop=(q == NQ - 1)), r=[w1s, xw], w=[pp[d * 2 + half]])
                    for j in range(2):
                        for half in range(2):
                            V("pe", lambda e: e.matmul(pp[4 + j * 2 + half][:], g1s[:, q, j * 128:(j + 1) * 128], xg[:, half * 512:(half + 1) * 512],
                                                       start=(q == 0), stop=(q == NQ - 1)), r=[g1s, xg], w=[pp[4 + j * 2 + half]])
                for d in range(2):
                    for half in range(2):
                        V("act", lambda e: e.activation(tw[d][:, half * 512:(half + 1) * 512], pp[d * 2 + half][0:96, :], AF.Tanh),
                          r=[pp[d * 2 + half]], w=[tw[d]])
                for j in range(2):
                    for half in range(2):
                        V("act", lambda e: e.activation(sgl[:, j, half * 512:(half + 1) * 512], pp[4 + j * 2 + half][:], AF.Sigmoid),
                          r=[pp[4 + j * 2 + half]], w=[sgl])
                for q in range(NQ):
                    xa = xs2[q % 4]
                    mixes(q, [4], [(xa, xa[:])], hp, dp, dn, tmpx)
                    for d in range(2):
                        for half in range(2):
                            V("pe", lambda e: e.matmul(pp[d * 2 + half][0:96, :], a1s[:, q, d, :], xa[:, half * 512:(half + 1) * 512],
                                                       start=(q == 0), stop=(q == NQ - 1)), r=[a1s, xa], w=[pp[d * 2 + half]])
                for d in range(2):
                    for half in range(2):
                        V("act", lambda e: e.activation(ta[d][:, half * 512:(half + 1) * 512], pp[d * 2 + half][0:96, :], AF.Copy),
                          r=[pp[d * 2 + half]], w=[ta[d]])
                k.barrier()
            with contextlib.ExitStack() as s2:
                xs_all = bfA
                xsv = [k.view(xs_all, "xs%d" % q) for q in range(NQ)]
                wpc = [k.sb([128, NQ, 512], BF16, "wpc", s2) for _ in range(2)]
                stg = [k.sb([128, NT], F32, "stg", s2) for _ in range(2)]
                pj = [k.ps([128, 512], F32, "pj", s2) for _ in range(4)]
                npc = 0; nst = 0; npj = 0
                for p_, (mi, wdram) in enumerate(((0, w_r_d), (2, w_k_d), (3, w_v_d))):
                    for q in range(NQ):
                        mixes(q, [mi], [(xsv[q], xs_all[:, q, :])], hp, dp, dn, tmpx)
                    wv = wdram.rearrange("(c p) n -> p c n", p=128)
                    for n_ in range(4):
                        wb = wpc[npc % 2]; npc += 1
                        k.dma("pool", wb[:], wv[:, :, n_ * 512:(n_ + 1) * 512], writes=[wb])
                        for jj in range(4):
                            qo = n_ * 4 + jj
                            sg_ = stg[nst % 2]; nst += 1
                            for half in range(2):
                                pz = pj[npj % 4]; npj += 1
                                for c in range(NQ):
                                    V("pe", lambda e: e.matmul(pz[:], wb[:, c, jj * 128:(jj + 1) * 128], xs_all[:, c, half * 512:(half + 1) * 512],
                                                               start=(c == 0), stop=(c == NQ - 1)), r=[wb, xsv[c]], w=[pz])
                                if half == 0:
                                    V("act", lambda e: e.activation(sg_[:, 0:512], pz[:], AF.Copy), r=[pz], w=[sg_])
                                else:
                                    V("dve", lambda e: e.tensor_copy(sg_[:, 512:1024], pz[:]), r=[pz], w=[sg_])
                            k.dma("sp", rkv_d[p_, qo], sg_[:], reads=[sg_])
                k.barrier()
        return st, tw, ta, sgl, negw0, omka

    def rwkv_scan(l, st, tw, ta, sgl, negw0, omka):
        z_all = bfA
        zv = [k.view(z_all, "z%d" % q) for q in range(NQ)]
        s3 = contextlib.ExitStack()
        F = lambda nm: k.sb([128, NT], F32, nm, s3)
        XV = lambda i, nm: TV(xT_all[:, i, :], nm)
        XV2 = lambda i, nm: TV(xT_all[:, i:i + 2, :], nm)
        KtRt = [XV2(0, "KtRt0"), XV2(2, "KtRt1")]
        AhKh = [XV2(4, "AhKh0"), XV2(6, "AhKh1")]
        r_, kr, v_ = XV(8, "r_"), XV(9, "kr"), XV(10, "v_")
        g_q, kap = XV(11, "g_q"), XV(12, "kap")
        lw, a_, cum = XV(13, "lw"), XV(14, "a_"), XV(15, "cum")
        Ep, Em, Ex, t0 = F("Ep"), F("Em"), F("Ex"), F("t0")
        yd = [F("y0"), F("y1")]
        glast = [k.sb([128, NCH], F32, "glast", s3) for _ in range(2)]
        w2s = k.sb([96, 2, 128], BF16, "w2s", s3); a2s = k.sb([96, 2, 128], BF16, "a2s", s3)
        g2s = k.sb([128, 2, 128], BF16, "g2s", s3)
        pbig = [k.ps([128, 512], F32, "pbig", s3) for _ in range(2)]
        pbon = [k.ps([128, 512], F32, "pbon", s3) for _ in range(2)]
        ps12 = [k.ps([128, 512], F32, "ps12", s3) for _ in range(2)]
        ptok = [k.ps([128, 512], F32, "ptok", s3) for _ in range(1)]
        psm_h = [k.ps([128, 512], F32, "psm", s3) for _ in range(1)]
        psm = [pbig[0], pbig[1], pbon[0], pbon[1], psm_h[0]]
        ring = {"psm": 0, "sb": 0, "big": 0}
        sbr = [k.sb([128, 128], F32, "sbr", s3) for _ in range(40)]
        def SB():
            t = sbr[ring["sb"] % len(sbr)]; ring["sb"] += 1; return t
        def PS():
            t = psm[ring["psm"] % len(psm)]; ring["psm"] += 1; return t, t.h[:, 0:128]
        def PB():
            t = pbig[ring["big"] % 2]; ring["big"] += 1; return t
        KR = [[k.sb([128, 2, 2, CH], F32, "KR", s3) for _ in range(2)] for _ in range(2)]
        AK = [[k.sb([128, 2, 2, CH], F32, "AK", s3) for _ in range(2)] for _ in range(2)]
        Vb = [k.sb([128, 2, CH], F32, "Vb", s3) for _ in range(2)]
        tokv = [k.sb([128, 128], F32, "tokv", s3) for _ in range(2)]
        tokak = [[k.sb([128, 2, 128], F32, "tokak", s3) for _ in range(2)] for _ in range(2)]
        SC1 = [[k.sb([128, 256], F32, "SC1", s3) for _ in range(2)] for _ in range(2)]
        SC2 = [[k.sb([128, 256], F32, "SC2", s3) for _ in range(2)] for _ in range(2)]
        Pst = [[k.sb([128, 128], F32, "P", s3) for _ in range(3)] for _ in range(2)]
        sbd = [k.sb([128, 128], F32, "sbd", s3) for _ in range(2)]
        snap = [k.sb([128, 128], F32, "snap", s3) for _ in range(4)]
        nsnap = [0]
        for d in range(2):
            V("pool", lambda e: e.memset(sbd[d][:], 0.0), w=[sbd[d]])
        H = lambda half: slice(half * 512, (half + 1) * 512)

        for q in range(NQ if dbg != "scan1" else 1):
            qs = slice(q * 128, (q + 1) * 128)
            k.dma("sp", r_[:], rkv_d[0, q], writes=[r_])
            k.dma("sp", kr[:], rkv_d[1, q], writes=[kr])
            k.dma("sp", v_[:], rkv_d[2, q], writes=[v_])
            k.dma("pool", w2s[:], w2_d.rearrange("d k n -> k d n")[:, :, qs], writes=[w2s])
            k.dma("pool", a2s[:], a2_d.rearrange("d k n -> k d n")[:, :, qs], writes=[a2s])
            k.dma("pool", g2s[:], g2_d.rearrange("(c p) n -> p c n", p=128)[:, :, qs], writes=[g2s])
            Pcur = [None, None]
            for d in range(2):
                for hh in range(2):
                    k.dma("sp", sbd[d][hh * 64:(hh + 1) * 64, hh * 64:(hh + 1) * 64], s0_d[d, 2 * q + hh], writes=[sbd[d]])
                pt_, pap = PS()
                V("pe", lambda e: e.transpose(pap, sbd[d][:], ident[:]), r=[sbd[d], ident], w=[pt_])
                Pcur[d] = Pst[d][0]
                V("act", lambda e: e.activation(Pcur[d][:], pap, AF.Copy), r=[pt_], w=[Pcur[d]])
            for half in range(2):
                pb = PB()
                for c in range(2):
                    V("pe", lambda e: e.matmul(pb[:], g2s[:, c, :], sgl[:, c, H(half)], start=(c == 0), stop=(c == 1)), r=[g2s, sgl], w=[pb])
                V("act", lambda e: e.activation(g_q[:, H(half)], pb[:], AF.Copy), r=[pb], w=[g_q])
            V("dve", lambda e: e.tensor_scalar(kap[:], kr[:], C("k_k", q), None, op0=ALU.mult), r=[kr, cols], w=[kap])
            V("pool", lambda e: e.tensor_tensor(t0[:], kap[:], kap[:], op=ALU.mult), r=[kap], w=[t0])
            for half in range(2):
                pb = PB()
                V("pe", lambda e: e.matmul(pb[:], blk1[:], t0[:, H(half)], start=True, stop=True), r=[blk1, t0], w=[pb])
                V("dve", lambda e: e.tensor_scalar(Ex[:, H(half)], pb[:], 1e-24, None, op0=ALU.max), r=[pb], w=[Ex])
                V("act", lambda e: e.activation(Ex[:, H(half)], Ex[:, H(half)], AF.Ln), r=[Ex], w=[Ex])
                V("act", lambda e: e.activation(Ex[:, H(half)], Ex[:, H(half)], AF.Exp, scale=-0.5), r=[Ex], w=[Ex])
            V("dve", lambda e: e.tensor_tensor(kap[:], kap[:], Ex[:], op=ALU.mult), r=[kap, Ex], w=[kap])
            for d in range(2):
                for half in range(2):
                    pb = PB()
                    V("pe", lambda e: e.matmul(pb[:], w2s[:, d, :], tw[d][:, H(half)], start=True, stop=True), r=[w2s, tw[d]], w=[pb])
                    V("act", lambda e: e.activation(lw[:, H(half)], pb[:], AF.Exp, bias=negw0[:, d, q:q + 1], scale=-1.0), r=[pb, negw0], w=[lw])
                    pb2 = PB()
                    V("pe", lambda e: e.matmul(pb2[:], a2s[:, d, :], ta[d][:, H(half)], start=True, stop=True), r=[a2s, ta[d]], w=[pb2])
                    V("act", lambda e: e.activation(a_[:, H(half)], pb2[:], AF.Sigmoid, bias=C("a0%d" % d, q), scale=1.0), r=[pb2, cols], w=[a_])
                V("dve", lambda e: e.tensor_scalar(lw[:], lw[:], 1.0, None, op0=ALU.add), r=[lw], w=[lw])
                V("act", lambda e: e.activation(lw[:], lw[:], AF.Ln), r=[lw], w=[lw])
                V("dve", lambda e: e.tensor_scalar(lw[:], lw[:], -1.0, -0.5, op0=ALU.mult, op1=ALU.add), r=[lw], w=[lw])
                V("act", lambda e: e.activation(lw[:], lw[:], AF.Exp), r=[lw], w=[lw])
                V("dve", lambda e: e.tensor_scalar(lw[:], lw[:], -1.0, None, op0=ALU.mult), r=[lw], w=[lw])
                V("dve", lambda e: e.tensor_scalar(t0[:], a_[:], C("k_a", q), omka[:, q:q + 1], op0=ALU.mult, op1=ALU.add), r=[a_, cols, omka], w=[t0])
                V("dve", lambda e: e.tensor_tensor(AhKh[d][:, 1, :], t0[:], kr[:], op=ALU.mult), r=[t0, kr], w=[AhKh[d]])
                V("pool", lambda e: e.tensor_tensor(AhKh[d][:, 0, :], kap[:], a_[:], op=ALU.mult), r=[kap, a_], w=[AhKh[d]])
                V("pool", lambda e: e.tensor_tensor(t0[:], r_[:], AhKh[d][:, 1, :], op=ALU.mult), r=[r_, AhKh[d], t0], w=[t0])
                V("pool", lambda e: e.tensor_scalar(t0[:], t0[:], C("r_k", q), None, op0=ALU.mult), r=[t0, cols], w=[t0])
                for half in range(2):
                    V("pe", lambda e: e.matmul(pbon[half][:], blk1[:], t0[:, H(half)], start=(d == 0), stop=(d == 1)), r=[blk1, t0], w=[pbon[half]])
                V("dve", lambda e: e.tensor_tensor_scan(cum[:], cmask[:].rearrange("p c t -> p (c t)"), lw[:], 0.0, op0=ALU.mult, op1=ALU.add),
                  r=[cmask, lw], w=[cum])
                if d == 1:
                    c3 = cum[:].rearrange("p (c t) -> p c t", t=CH)
                    V("dve", lambda e: e.tensor_copy(glast[d][:], c3[:, :, CH - 1]), r=[cum], w=[glast[d]])
                    V("dve", lambda e: e.tensor_tensor(cum[:], lw[:], cum[:], op=ALU.subtract), r=[lw, cum], w=[cum])
                    V("dve", lambda e: e.tensor_tensor(c3, c3, glast[d][:].unsqueeze(2).broadcast_to([128, NCH, CH]), op=ALU.add),
                      r=[cum, glast[d]], w=[cum])
                V("act", lambda e: e.activation(Ep[:], cum[:], AF.Exp), r=[cum], w=[Ep])
                V("act", lambda e: e.activation(Em[:], cum[:], AF.Exp, scale=-1.0), r=[cum], w=[Em])
                V("pool", lambda e: e.tensor_tensor(t0[:], cum[:], lw[:], op=ALU.subtract), r=[cum, lw], w=[t0])
                V("act", lambda e: e.activation(Ex[:], t0[:], AF.Exp), r=[t0], w=[Ex])
                e3 = Ep[:].rearrange("p (c t) -> p c t", t=CH)
                V("dve", lambda e: e.tensor_copy(glast[d][:], e3[:, :, CH - 1] if d == 0 else e3[:, :, 0]), r=[Ep], w=[glast[d]])
                V("dve", lambda e: e.tensor_tensor(KtRt[d][:, 0, :], kap[:], Ex[:], op=ALU.mult), r=[kap, Ex], w=[KtRt[d]])
                V("pool", lambda e: e.tensor_tensor(KtRt[d][:, 1, :], r_[:], Ep[:], op=ALU.mult), r=[r_, Ep], w=[KtRt[d]])
                V("dve", lambda e: e.tensor_tensor(AhKh[d][:, 0, :], AhKh[d][:, 0, :], Em[:], op=ALU.mult), r=[AhKh[d], Em], w=[AhKh[d]])
                V("pool", lambda e: e.tensor_tensor(AhKh[d][:, 1, :], AhKh[d][:, 1, :], Em[:], op=ALU.mult), r=[AhKh[d], Em], w=[AhKh[d]])
            for half in range(2):
                V("dve", lambda e: e.tensor_tensor(Ex[:, H(half)], pbon[half][:], v_[:, H(half)], op=ALU.mult), r=[pbon[half], v_], w=[Ex])
            bm4 = bmask[:].unsqueeze(1).broadcast_to([128, 2, 2, CH])
            for step in range(NCH):
                for d in range(2):
                    c = step if d == 0 else NCH - 1 - step
                    cs = slice(c * CH, (c + 1) * CH)
                    kr_t = KR[d][step % 2]; ak_t = AK[d][step % 2]
                    V("pool", lambda e: e.tensor_tensor(kr_t[:], KtRt[d][:, :, cs].unsqueeze(2).broadcast_to([128, 2, 2, CH]), bm4, op=ALU.mult),
                      r=[KtRt[d], bmask], w=[kr_t])
                    V("pool", lambda e: e.tensor_tensor(ak_t[:], AhKh[d][:, :, cs].unsqueeze(2).broadcast_to([128, 2, 2, CH]), bm4, op=ALU.mult),
                      r=[AhKh[d], bmask], w=[ak_t])
                    vb = Vb[d]; tv = tokv[d]
                    V("pool", lambda e: e.tensor_tensor(vb[:], v_[:, cs].unsqueeze(1).broadcast_to([128, 2, CH]), bmask[:], op=ALU.mult),
                      r=[v_, bmask], w=[vb])
                    KRf = lambda i: kr_t[:, i, :, :].rearrange("p a t -> p (a t)")
                    AKf = lambda i: ak_t[:, i, :, :].rearrange("p a t -> p (a t)")
                    KR2 = kr_t[:].rearrange("p i a t -> p (i a t)")
                    vbf = vb[:].rearrange("p a t -> p (a t)")
                    pk = ptok[0]
                    V("pe", lambda e: e.transpose(pk[:, 0:128], AKf(0), ident[:]), r=[ak_t, ident], w=[pk])
                    V("pe", lambda e: e.transpose(pk[:, 128:256], AKf(1), ident[:]), r=[ak_t, ident], w=[pk])
                    V("pe", lambda e: e.transpose(pk[:, 256:384], vbf, ident[:]), r=[vb, ident], w=[pk])
                    tak = tokak[d][step % 2]
                    V("act", lambda e: e.activation(tak[:].rearrange("p i t -> p (i t)"), pk[:, 0:256], AF.Copy), r=[pk], w=[tak])
                    V("act", lambda e: e.activation(tv[:], pk[:, 256:384], AF.Copy), r=[pk], w=[tv])
                    p12 = ps12[d]
                    V("pe", lambda e: e.matmul(p12[:, 0:256], AKf(0), KR2, start=True, stop=True), r=[ak_t, kr_t], w=[p12])
                    V("pe", lambda e: e.matmul(p12[:, 256:512], AKf(1), KR2, start=True, stop=True), r=[ak_t, kr_t], w=[p12])
                    p3t, p3 = PS()
                    V("pe", lambda e: e.matmul(p3, KRf(0), AKf(0), start=True, stop=True), r=[kr_t, ak_t], w=[p3t])
                    sc1 = SC1[d][step % 2]; sc2 = SC2[d][step % 2]
                    V("dve", lambda e: e.tensor_tensor(sc1[:], p12[:, 0:256], MK1[d][:], op=ALU.mult), r=[p12, MK1[d]], w=[sc1])
                    V("dve", lambda e: e.tensor_tensor(sc2[:], p12[:, 256:512], MK2[d][:], op=ALU.mult), r=[p12, MK2[d]], w=[sc2])
                    X = SB()
                    V("dve", lambda e: e.tensor_tensor(X[:], p3, MKA[d][:], op=ALU.mult), r=[p3t, MKA[d]], w=[X])
                    TT = SB()
                    V("pool", lambda e: e.tensor_tensor(TT[:], sc1[:, 0:128], ident[:], op=ALU.add), r=[sc1, ident], w=[TT])
                    XTt, XTap = sc1, sc1[:, 0:128]
                    for kq in range(1, 6):
                        pxt_, pxa = PS()
                        V("pe", lambda e: e.matmul(pxa, XTap, X[:], start=True, stop=True), r=[XTt, X], w=[pxt_])
                        X2 = SB()
                        V("act", lambda e: e.activation(X2[:], pxa, AF.Copy), r=[pxt_], w=[X2])
                        if kq < 5:
                            pyt_, pya = PS()
                            V("pe", lambda e: e.matmul(pya, X[:], XTap, start=True, stop=True), r=[X, XTt], w=[pyt_])
                            XT2 = SB()
                            V("dve", lambda e: e.tensor_copy(XT2[:], pya), r=[pyt_], w=[XT2])
                        ptt_, pta = PS()
                        V("pe", lambda e: e.matmul(pta, X2[:], TT[:], start=True, stop=True), r=[X2, TT], w=[ptt_])
                        TT2 = SB()
                        V("dve", lambda e: e.tensor_tensor(TT2[:], pta, TT[:], op=ALU.add), r=[ptt_, TT], w=[TT2])
                        X = X2; TT = TT2
                        if kq < 5:
                            XTt, XTap = XT2, XT2[:]
                    P = Pcur[d]
                    put_, pua = PS()
                    V("pe", lambda e: e.matmul(pua, sc2[:, 0:128], tv[:], start=True, stop=False), r=[sc2, tv], w=[put_])
                    V("pe", lambda e: e.matmul(pua, KRf(0), P[:], start=False, stop=True), r=[kr_t, P], w=[put_])
                    U = SB()
                    V("act", lambda e: e.activation(U[:], pua, AF.Copy), r=[put_], w=[U])
                    pzt_, pza = PS()
                    V("pe", lambda e: e.matmul(pza, TT[:], U[:], start=True, stop=True), r=[TT, U], w=[pzt_])
                    Z = SB()
                    V("act", lambda e: e.mul(Z[:], pza, -1.0), r=[pzt_], w=[Z])
                    pyt2, pya2 = PS()
                    V("pe", lambda e: e.matmul(pya2, P[:], KRf(1), start=True, stop=False), r=[P, kr_t], w=[pyt2])
                    V("pe", lambda e: e.matmul(pya2, Z[:], sc1[:, 128:256], start=False, stop=False), r=[Z, sc1], w=[pyt2])
                    V("pe", lambda e: e.matmul(pya2, tv[:], sc2[:, 128:256], start=False, stop=True), r=[tv, sc2], w=[pyt2])
                    V("dve", lambda e: e.tensor_copy(yd[d][0:64, cs], pya2[0:64, 0:64]), r=[pyt2], w=[yd[d]])
                    V("act", lambda e: e.activation(yd[d][64:128, cs], pya2[64:128, 64:128], AF.Copy), r=[pyt2], w=[yd[d]])
                    ppt_, ppa = PS()
                    V("pe", lambda e: e.matmul(ppa, ident[:], P[:], start=True, stop=False), r=[ident, P], w=[ppt_])
                    V("pe", lambda e: e.matmul(ppa, tak[:, 0, :], Z[:], start=False, stop=False), r=[tak, Z], w=[ppt_])
                    V("pe", lambda e: e.matmul(ppa, tak[:, 1, :], tv[:], start=False, stop=True), r=[tak, tv], w=[ppt_])
                    Pn = Pst[d][(step + 1) % 3]
                    V("dve", lambda e: e.tensor_scalar(Pn[:], ppa, glast[d][:, c:c + 1], None, op0=ALU.mult), r=[ppt_, glast[d]], w=[Pn])
                    Pcur[d] = Pn
                    if step % 4 == 3:
                        seq = c // 4
                        pst_, psa = PS()
                        V("pe", lambda e: e.transpose(psa, Pn[:], ident[:]), r=[Pn, ident], w=[pst_])
                        sn = snap[nsnap[0] % 4]; nsnap[0] += 1
                        V("act", lambda e: e.activation(sn[:], psa, AF.Copy), r=[pst_], w=[sn])
                        for hh in range(2):
                            k.dma("sp", ns_d[seq, d, 2 * q + hh], sn[hh * 64:(hh + 1) * 64, hh * 64:(hh + 1) * 64], reads=[sn])
                        if step < NCH - 1:
                            Pk = Pst[d][(step + 2) % 3]
                            V("dve", lambda e: e.tensor_scalar(Pk[:], Pn[:], keepc[:, d, c:c + 1], None, op0=ALU.mult), r=[Pn, keepc], w=[Pk])
                            Pcur[d] = Pk
            if dbg == "scan1":
                k.barrier()
                for i_, (tt_, ap_) in enumerate(((yd[0], yd[0][:]), (yd[1], yd[1][:]), (KtRt[1], KtRt[1][:, 0, :]), (KtRt[1], KtRt[1][:, 1, :]),
                                                 (AhKh[1], AhKh[1][:, 0, :]), (AhKh[1], AhKh[1][:, 1, :]), (lw, lw[:]), (kap, kap[:]), (g_q, g_q[:]),
                                                 (a_, a_[:]), (cum, cum[:]))):
                    k.dma("sp", dbg_d[i_], ap_, reads=[tt_])
                k.dma("sp", dbg_d[11, :, 0:NCH], glast[1][:], reads=[glast[1]])
                k.barrier()
            V("pool", lambda e: e.tensor_tensor(cum[:], yd[0][:], yd[1][:], op=ALU.add), r=[yd[0], yd[1]], w=[cum])
            V("act", lambda e: e.activation(t0[:], cum[:], AF.Square), r=[cum], w=[t0])
            for half in range(2):
                pm = PB(); p2 = PB()
                V("pe", lambda e: e.matmul(pm[:], blkm[:], cum[:, H(half)], start=True, stop=True), r=[blkm, cum], w=[pm])
                V("pe", lambda e: e.matmul(p2[:], blkm[:], t0[:, H(half)], start=True, stop=True), r=[blkm, t0], w=[p2])
                V("act", lambda e: e.activation(Ep[:, H(half)], pm[:], AF.Copy), r=[pm], w=[Ep])
                V("dve", lambda e: e.tensor_tensor(Em[:, H(half)], Ep[:, H(half)], Ep[:, H(half)], op=ALU.mult), r=[Ep], w=[Em])
                V("dve", lambda e: e.tensor_tensor(Em[:, H(half)], p2[:], Em[:, H(half)], op=ALU.subtract), r=[p2, Em], w=[Em])
            V("dve", lambda e: e.tensor_scalar(Em[:], Em[:], 0.0, GN_EPS, op0=ALU.max, op1=ALU.add), r=[Em], w=[Em])
            V("act", lambda e: e.activation(Em[:], Em[:], AF.Ln), r=[Em], w=[Em])
            V("act", lambda e: e.activation(Em[:], Em[:], AF.Exp, scale=-0.5), r=[Em], w=[Em])
            V("pool", lambda e: e.tensor_tensor(cum[:], cum[:], Ep[:], op=ALU.subtract), r=[cum, Ep], w=[cum])
            V("pool", lambda e: e.tensor_tensor(cum[:], cum[:], Em[:], op=ALU.mult), r=[cum, Em], w=[cum])
            V("dve", lambda e: e.tensor_scalar(cum[:], cum[:], C("gn_g", q), C("gn_b", q), op0=ALU.mult, op1=ALU.add), r=[cum, cols], w=[cum])
            V("pool", lambda e: e.tensor_tensor(cum[:], cum[:], Ex[:], op=ALU.add), r=[cum, Ex], w=[cum])
            V("dve", lambda e: e.tensor_tensor(z_all[:, q, :], cum[:], g_q[:], op=ALU.mult), r=[cum, g_q], w=[zv[q]])
        k.barrier()
        s3.close()
        if dbg == "scan1":
            st.close()
            return
        with contextlib.ExitStack() as s4:
            load_xT(s4)
            wpc = [k.sb([128, NQ, 512], BF16, "wo", s4) for _ in range(2)]
            pj = [k.ps([128, 512], F32, "pjo", s4) for _ in range(2)]
            wv = w_o_d.rearrange("(c p) n -> p c n", p=128)
            npj = 0
            for n_ in range(4):
                wb = wpc[n_ % 2]
                k.dma("pool", wb[:], wv[:, :, n_ * 512:(n_ + 1) * 512], writes=[wb])
                for jj in range(4):
                    qo = n_ * 4 + jj
                    V("pool", lambda e: e.tensor_scalar(XT(qo), XT(qo), ALPHA, None, op0=ALU.mult), r=[xT[qo]], w=[xT[qo]])
                    for half in range(2):
                        pz = pj[npj % 2]; npj += 1
                        for c in range(NQ):
                            V("pe", lambda e: e.matmul(pz[:], wb[:, c, jj * 128:(jj + 1) * 128], z_all[:, c, H(half)],
                                                       start=(c == 0), stop=(c == NQ - 1)), r=[wb, zv[c]], w=[pz])
                        V("dve", lambda e: e.scalar_tensor_tensor(xT_all[:, qo, H(half)], pz[:], ADA(l, 2, qo), xT_all[:, qo, H(half)],
                                                                  op0=ALU.mult, op1=ALU.add), r=[pz, ada, xT[qo]], w=[xT[qo]])
            k.barrier()
        st.close()
        with contextlib.ExitStack() as s5:
            layer_norm(l, 0, s5)
            k.barrier()

    def pool_layer(l=1):
        H = lambda half: slice(half * 512, (half + 1) * 512)
        with contextlib.ExitStack() as s:
            htok = k.sb([128, 8, D], BF16, "htok", s)
            hb = [k.sb([128, NT], BF16, "hb", s) for _ in range(2)]
            hf = [k.sb([128, 512], F32, "hf", s) for _ in range(2)]
            tq = [k.sb([128, 512], F32, "tq", s) for _ in range(2)]
            pT_all = bfA
            pTv = [k.view(pT_all, "pT%d" % q) for q in range(NQ)]
            ic = k.sb([128, 4, NT], F32, "ic", s)
            k.dma("sp", ic[:], poolic_d, writes=[ic])
            psg = k.sb([128, 16], F32, "psg", s)
            V("dve", lambda e: e.tensor_tensor(psg[:], C("pool_scale", 0, 16), ada[:, l, 32:48], op=ALU.mult), r=[cols, ada], w=[psg])
            ptb = [k.ps([128, 8, 128], BF16, "ptb", s) for _ in range(2)]
            pm_ = [k.ps([128, 512], F32, "pm", s) for _ in range(2)]
            py_ = [k.ps([128, 512], F32, "py", s) for _ in range(2)]
            for q in range(NQ):
                h_ = hb[q % 2]; pt = ptb[q % 2]
                V("dve", lambda e: e.tensor_scalar(h_[:], XT(q), OPSC(l, 0, q), ADA(l, 0, q), op0=ALU.mult, op1=ALU.add),
                  r=[xT[q], opsc, ada], w=[h_])
                for tt in range(8):
                    V("pe", lambda e: e.transpose(pt[:, tt, :], h_[:, tt * 128:(tt + 1) * 128], identb[:]), r=[h_, identb], w=[pt])
                V("act", lambda e: e.activation(htok[:, :, q * 128:(q + 1) * 128], pt[:], AF.Copy), r=[pt], w=[htok])
            MT = [k.sb([128, 8, NT], BF16, "MT", s) for _ in range(1)]
            wpl = [k.sb([128, 4, 512], BF16, "wpl", s) for _ in range(2)]
            n1 = 0
            for g in range(4):
                mt = MT[0]; wp = wpl[g % 2]
                k.dma("sp", mt[:], poolMT_d[g].rearrange("(c p) t -> p c t", p=128), writes=[mt])
                k.dma("pool", wp[:], pool_w_d[g].rearrange("(c p) n -> p c n", p=128), writes=[wp])
                for qq in range(4):
                    q = 4 * g + qq
                    for half in range(2):
                        pm = pm_[n1 % 2]; hf_ = hf[n1 % 2]; tq_ = tq[n1 % 2]; n1 += 1
                        for st_ in range(8):
                            V("pe", lambda e: e.matmul(pm[:], htok[:, st_, q * 128:(q + 1) * 128], mt[:, st_, H(half)],
                                                       start=(st_ == 0), stop=(st_ == 7)), r=[htok, mt], w=[pm])
                        V("dve", lambda e: e.tensor_tensor(tq_[:], pm[:], ic[:, g, H(half)], op=ALU.mult), r=[pm, ic], w=[tq_])
                        V("pool", lambda e: e.tensor_scalar(hf_[:], xT_all[:, q, H(half)], OPSC(l, 0, q), ADA(l, 0, q), op0=ALU.mult, op1=ALU.add),
                          r=[xT[q], opsc, ada], w=[hf_])
                        V("dve", lambda e: e.tensor_tensor(pT_all[:, q, H(half)], tq_[:], hf_[:], op=ALU.subtract), r=[tq_, hf_], w=[pTv[q]])
                for dt_ in range(4):
                    qo = 4 * g + dt_
                    V("pool", lambda e: e.tensor_scalar(XT(qo), XT(qo), ALPHA, None, op0=ALU.mult), r=[xT[qo]], w=[xT[qo]])
                    for half in range(2):
                        py = py_[n1 % 2]; n1 += 1
                        for ct in range(4):
                            V("pe", lambda e: e.matmul(py[:], wp[:, ct, dt_ * 128:(dt_ + 1) * 128], pT_all[:, 4 * g + ct, H(half)],
                                                       start=(ct == 0), stop=(ct == 3)), r=[wp, pTv[4 * g + ct]], w=[py])
                        V("dve", lambda e: e.scalar_tensor_tensor(xT_all[:, qo, H(half)], py[:], psg[:, qo:qo + 1], xT_all[:, qo, H(half)],
                                                                  op0=ALU.mult, op1=ALU.add), r=[py, psg, xT[qo]], w=[xT[qo]])
            k.barrier()
        with contextlib.ExitStack() as s5:
            layer_norm(l, 0, s5)
            k.barrier()

    def store_out():
        with contextlib.ExitStack() as s:
            ytok = [k.sb([128, D], F32, "ytok", s) for _ in range(2)]
            tp = [k.ps([128, 4, 128], F32, "tpo", s) for _ in range(2)]
            n = 0
            for tt in range(8):
                yb = ytok[tt % 2]
                for qg in range(4):
                    p = tp[n % 2]; n += 1
                    for j in range(4):
                        q = qg * 4 + j
                        V("pe", lambda e: e.transpose(p[:, j, :], xT_all[:, q, tt * 128:(tt + 1) * 128], ident[:]), r=[xT[q], ident], w=[p])
                    if n % 2:
                        V("act", lambda e: e.activation(yb[:, qg * 512:(qg + 1) * 512], p[:].rearrange("p a b -> p (a b)"), AF.Copy), r=[p], w=[yb])
                    else:
                        V("dve", lambda e: e.tensor_copy(yb[:, qg * 512:(qg + 1) * 512], p[:].rearrange("p a b -> p (a b)")), r=[p], w=[yb])
                k.dma("sp", y_d[tt * 128:(tt + 1) * 128, :], yb[:], reads=[yb])

    def dump_dbg():
        for q in range(NQ):
            k.dma("sp", dbg_d[q], XT(q), reads=[xT[q]])

    stop = dbg
    if stop == "ada":
        with contextlib.ExitStack() as s_:
            load_xT(s_); k.barrier()
        for q in range(6):
            V("dve", lambda e: e.tensor_copy(xT_all[:, q, 0:96], ada[:, 0, :]), r=[ada, xT[q]], w=[xT[q]])
            V("dve", lambda e: e.tensor_copy(xT_all[:, q, 96:192], ada[:, 1, :]), r=[ada, xT[q]], w=[xT[q]])
        dump_dbg()
    elif stop == "mix":
        st = rwkv_layer(0)[0]
        st.close()
    elif stop == "p2":
        st, tw, ta, sgl, negw0, omka = rwkv_layer(0)
        for p_ in range(3):
            for q in range(4):
                k.dma("sp", XT(p_ * 4 + q), rkv_d[p_, q * 5], writes=[xT[p_ * 4 + q]])
        V("dve", lambda e: e.tensor_copy(xT_all[0:96, 12, :], tw[0][:]), r=[tw[0]], w=[xT[12]])
        V("dve", lambda e: e.tensor_copy(xT_all[0:96, 13, :], ta[1][:]), r=[ta[1]], w=[xT[13]])
        V("dve", lambda e: e.tensor_copy(xT_all[:, 14, :], sgl[:, 1, :]), r=[sgl], w=[xT[14]])
        k.barrier()
        st.close()
        dump_dbg()
    elif stop == "scan1":
        st, tw, ta, sgl, negw0, omka = rwkv_layer(0)
        rwkv_scan(0, st, tw, ta, sgl, negw0, omka)
    elif stop == "rwkv":
        st, tw, ta, sgl, negw0, omka = rwkv_layer(0)
        rwkv_scan(0, st, tw, ta, sgl, negw0, omka)
        dump_dbg()
    else:
        st, tw, ta, sgl, negw0, omka = rwkv_layer(0)
        rwkv_scan(0, st, tw, ta, sgl, negw0, omka)
        with contextlib.ExitStack() as s_:
            moe(0, s_)
        with contextlib.ExitStack() as s_:
            layer_norm(0, 1, s_); k.barrier()
        if stop == "l0":
            dump_dbg()
        else:
            pool_layer(1)
            if stop == "pool":
                dump_dbg()
            else:
                with contextlib.ExitStack() as s_:
                    moe(1, s_)
                with contextlib.ExitStack() as s_:
                    layer_norm(1, 1, s_); k.barrier()
    store_out()
    k.finish()
    k.close()
    nc._in_names = in_names
    return nc


def _pool_consts(is_sample):
    MT = np.zeros((4, NT, NT), np.float32)
    ic = np.zeros((4, NT), np.float32)
    for gi, w in enumerate((2, 4, 8, 16)):
        if not is_sample:
            L = 256
            for t in range(NT):
                base = (t // L) * L; tl = t % L
                lo = min(max(tl - w // 2, 0), L); hi = min(max(tl - w // 2 + w, 0), L)
                MT[gi, base + lo:base + hi, t] = 1.0
                ic[gi, t] = 1.0 / (hi - lo)
        else:
            R, W = 16, 64
            for t in range(NT):
                r, c = t // W, t % W
                rlo = min(max(r - w // 2, 0), R); rhi = min(max(r - w // 2 + w, 0), R)
                clo = min(max(c - w // 2, 0), W); chi = min(max(c - w // 2 + w, 0), W)
                for rr in range(rlo, rhi):
                    MT[gi, rr * W + clo:rr * W + chi, t] = 1.0
                ic[gi, t] = 1.0 / ((rhi - rlo) * (chi - clo))
    return MT.astype(ml_dtypes.bfloat16), np.ascontiguousarray(np.broadcast_to(ic[None], (128, 4, NT))).astype(np.float32)


_PROG = {}


def kernel(**inp):
    dbg = inp.pop("_dbg", None)
    inp["_cores"] = inp.pop("_cores", None)
    f32 = lambda a: np.ascontiguousarray(np.asarray(a, np.float32))
    colsa = np.zeros((128, NCOL), np.float32)
    def put(name, v, w=16):
        colsa[:, COLOFF[name]:COLOFF[name] + w] = _colvec(v)
    for l in range(2):
        put("b_ada%d" % l, inp["b_ada"][l], 96)
        for s in range(2):
            put("ln_g%d%d" % (l, s), inp["ln_g"][l, s]); put("ln_b%d%d" % (l, s), inp["ln_b"][l, s])
    for i in range(6):
        put("mp%d" % i, inp["rw_mix_prev"][0, i]); put("mn%d" % i, inp["rw_mix_next"][0, i])
    for d in range(2):
        put("w0%d" % d, inp["rw_w0"][0, d]); put("a0%d" % d, inp["rw_a0"][0, d])
    put("k_k", inp["rw_k_k"][0]); put("k_a", inp["rw_k_a"][0]); put("r_k", np.asarray(inp["rw_r_k"][0]).reshape(-1))
    put("gn_g", inp["rw_gn_g"][0]); put("gn_b", inp["rw_gn_b"][0]); put("pool_scale", inp["pool_scale"][0])
    rbias = np.ascontiguousarray(np.broadcast_to(np.asarray(inp["moe_router_bias"], np.float32)[None], (128, 2, NE)))
    shared = {
        "cols": colsa, "rbias": rbias, "w_ada": f32(inp["w_ada"]),
        "rw_w_r": f32(inp["rw_w_r"][0]), "rw_w_k": f32(inp["rw_w_k"][0]), "rw_w_v": f32(inp["rw_w_v"][0]),
        "rw_w_o": f32(inp["rw_w_o"][0]), "rw_w1": f32(inp["rw_w1"][0]), "rw_w2": f32(inp["rw_w2"][0]),
        "rw_a1": f32(inp["rw_a1"][0]), "rw_a2": f32(inp["rw_a2"][0]), "rw_g1": f32(inp["rw_g1"][0]),
        "rw_g2": f32(inp["rw_g2"][0]), "pool_w": f32(inp["pool_w"][0]), "moe_router": f32(inp["moe_router"]),
        "moe_w_gate": f32(inp["moe_w_gate"]), "moe_w_up": f32(inp["moe_w_up"]), "moe_w_down": f32(inp["moe_w_down"]),
        "moe_ws_gate": f32(inp["moe_ws_gate"]), "moe_ws_up": f32(inp["moe_ws_up"]), "moe_ws_down": f32(inp["moe_ws_down"]),
    }
    pc = {False: _pool_consts(False), True: _pool_consts(True)}
    x_prompt = f32(inp["x_prompt"]); x_sample = f32(inp["x_sample"])
    c = f32(inp["c"]); c_ctx = f32(inp["c_ctx"]); state = f32(inp["state_rwkv"])
    in_maps = []
    for core in range(8):
        samp = core >= 4
        m = dict(shared)
        if not samp:
            m["x"] = np.ascontiguousarray(x_prompt[4 * core:4 * core + 4].reshape(NT, D))
            m["cond"] = _colvec(c_ctx)
            m["s0"] = np.zeros((2, 32, 64, 64), np.float32)
            keep = np.ones((2, NCH), np.float32)
            keep[0, [3, 7, 11]] = 0.0; keep[1, [12, 8, 4]] = 0.0
            tm = np.ones((2, NT), np.float32)
            tm[0, 0::256] = 0.0; tm[1, 255::256] = 0.0
        else:
            b = core - 4
            m["x"] = np.ascontiguousarray(x_sample[b])
            m["cond"] = _colvec(c[b])
            m["s0"] = np.ascontiguousarray(state[b, 0])
            keep = np.ones((2, NCH), np.float32)
            tm = np.ones((2, NT), np.float32)
            tm[0, 0] = 0.0; tm[1, NT - 1] = 0.0
        m["keepc"] = np.ascontiguousarray(np.broadcast_to(keep[None], (128, 2, NCH)))
        m["tmask"] = np.ascontiguousarray(np.broadcast_to(tm[None], (128, 2, NT)))
        m["poolMT"], m["poolic"] = pc[samp]
        in_maps.append(m)
    if dbg not in _PROG:
        _PROG[dbg] = build_program(dbg)
    names = set(_PROG[dbg]._in_names)
    in_maps = [{kk: vv for kk, vv in m.items() if kk in names} for m in in_maps]
    import time as _time
    _t0 = _time.time()
    if dbg is not None and inp.get("_cores") is not None:
        cl = inp["_cores"]
        res = run_bass_kernel_spmd(_PROG[dbg], [in_maps[c_] for c_ in cl], core_ids=list(range(len(cl))))
        return None, {c_: res.results[i]["dbg"] for i, c_ in enumerate(cl)}, {c_: res.results[i]["ns"] for i, c_ in enumerate(cl)}
    res = run_bass_kernel_spmd(_PROG[dbg], in_maps, core_ids=list(range(8)))
    R = res.results
    y_prompt = np.stack([R[cidx]["y"] for cidx in range(4)]).reshape(16, 256, D).astype(np.float32)
    y_sample = np.stack([R[cidx]["y"] for cidx in range(4, 8)]).reshape(4, NT, D).astype(np.float32)
    ns = np.concatenate([R[cidx]["ns"] for cidx in range(4)], axis=0)
    new_state = np.ascontiguousarray(ns[:, None]).astype(np.float32)
    if dbg is not None:
        return (y_prompt, y_sample, new_state), [R[cidx]["dbg"] for cidx in range(8)]
    return (y_prompt, y_sample, new_state)
```

```python
import contextlib
import numpy as np
import ml_dtypes
import concourse.bass as bass
import concourse.mybir as mybir
from concourse.bass_utils import run_bass_kernel_spmd

F32 = mybir.dt.float32
BF16 = mybir.dt.bfloat16
AF = mybir.ActivationFunctionType
ALU = mybir.AluOpType
AX = mybir.AxisListType

D = 2048
NT = 1024
NQ = 16
NE = 64
ALPHA = (2 * 2) ** 0.25
LN_EPS = 1e-5
GN_EPS = 64e-5
CH = 64
NCH = NT // CH


class T:
    __slots__ = ("h", "w", "r", "name")

    def __init__(self, h, name):
        self.h = h
        self.name = name
        self.w = None
        self.r = []

    def __getitem__(self, idx):
        return self.h[idx]


class TV(T):
    __slots__ = ("ap",)

    def __init__(self, ap, name):
        T.__init__(self, None, name)
        self.ap = ap

    def __getitem__(self, idx):
        return self.ap[idx]


class KB:
    ENG = ("pe", "act", "dve", "pool", "sp")

    def __init__(self, nc, n_dma_sems=32):
        self.nc = nc
        self.es = contextlib.ExitStack()
        self.e = {"pe": nc.tensor, "act": nc.scalar, "dve": nc.vector,
                  "pool": nc.gpsimd, "sp": nc.sync}
        self.sem = {k: self.es.enter_context(nc.semaphore("s_" + k)) for k in self.ENG}
        self.cnt = {k: 0 for k in self.ENG}
        self.seen = {k: {} for k in self.ENG}
        self.dsem = [self.es.enter_context(nc.semaphore("d%d" % i)) for i in range(n_dma_sems)]
        self.dval = [0] * n_dma_sems
        half = n_dma_sems // 2
        self.dpool = {"sp": list(range(0, half)), "act": list(range(0, half)), "pool": list(range(half, n_dma_sems))}
        self.dnext = {"sp": 0, "act": 0, "pool": 0}
        self.nalloc = 0
        self.ninst = 0

    def sb(self, shape, dt=F32, name=None, stack=None):
        self.nalloc += 1
        name = (name or "t") + "_%d" % self.nalloc
        h = (stack or self.es).enter_context(self.nc.sbuf_tensor(name, list(shape), dt))
        return T(h, name)

    def ps(self, shape, dt=F32, name=None, stack=None):
        self.nalloc += 1
        name = (name or "p") + "_%d" % self.nalloc
        h = (stack or self.es).enter_context(self.nc.psum_tensor(name, list(shape), dt))
        return T(h, name)

    def view(self, t, name=None):
        return T(t.h, name or t.name + "_v")

    def _need(self, eng, dep):
        if dep is None:
            return
        if dep[0] == "e":
            _, de, val = dep
            key = de
            sem = self.sem[de]
        else:
            _, si, val = dep
            key = "d%d" % si
            sem = self.dsem[si]
        if self.seen[eng].get(key, 0) >= val:
            return
        self.e[eng].wait_ge(sem, val)
        self.ninst += 1
        self.seen[eng][key] = val

    def _deps(self, eng, reads, writes):
        for t in reads:
            if eng == "pe" and t.w is not None and t.w[0] == "e" and t.w[1] == "pe":
                continue
            self._need(eng, t.w)
        for t in writes:
            same_pe = eng == "pe"
            if not (same_pe and t.w is not None and t.w[0] == "e" and t.w[1] == "pe"):
                self._need(eng, t.w)
            for d in t.r:
                if same_pe and d[0] == "e" and d[1] == "pe":
                    continue
                self._need(eng, d)

    def op(self, eng, fn, reads=(), writes=()):
        self._deps(eng, reads, writes)
        ins = fn(self.e[eng])
        self.cnt[eng] += 1
        ins.then_inc(self.sem[eng], 1)
        self.ninst += 1
        me = ("e", eng, self.cnt[eng])
        for t in reads:
            t.r.append(me)
            if len(t.r) > 48:
                best = {}
                for d in t.r:
                    kk = (d[0], d[1])
                    if kk not in best or best[kk][2] < d[2]:
                        best[kk] = d
                t.r = list(best.values())
        for t in writes:
            t.w = me
            t.r = []
        return ins

    def dma(self, eng, out, in_, reads=(), writes=(), **kw):
        self._deps(eng, reads, writes)
        pool_ = self.dpool[eng]
        qk = "sp" if eng == "act" else eng
        si = pool_[self.dnext[qk] % len(pool_)]
        self.dnext[qk] += 1
        self._need(eng, ("d", si, self.dval[si]))
        ins = self.e[eng].dma_start(out=out, in_=in_, **kw)
        self.dval[si] += 16
        ins.then_inc(self.dsem[si], 16)
        self.ninst += 1
        me = ("d", si, self.dval[si])
        for t in reads:
            t.r.append(me)
        for t in writes:
            t.w = me
            t.r = []
        return me

    def barrier(self):
        for eng in self.ENG:
            for other in self.ENG:
                if other != eng and self.cnt[other] > 0:
                    self._need(eng, ("e", other, self.cnt[other]))
            for si in range(len(self.dsem)):
                if self.dval[si] > 0:
                    self._need(eng, ("d", si, self.dval[si]))

    def finish(self, eng="sp"):
        for si in range(len(self.dsem)):
            if self.dval[si] > 0:
                self._need(eng, ("d", si, self.dval[si]))
        for other in self.ENG:
            if other != eng and self.cnt[other] > 0:
                self._need(eng, ("e", other, self.cnt[other]))

    def close(self):
        self.es.close()

def _col_layout():
    off = {}
    n = 0
    def add(name, w=16):
        nonlocal n
        off[name] = n
        n += w
    for l in range(2):
        add("b_ada%d" % l, 96)
    for l in range(2):
        for s in range(2):
            add("ln_g%d%d" % (l, s)); add("ln_b%d%d" % (l, s))
    for i in range(6):
        add("mp%d" % i); add("mn%d" % i)
    for d in range(2):
        add("w0%d" % d); add("a0%d" % d)
    for nm in ("k_k", "k_a", "r_k", "gn_g", "gn_b", "pool_scale"):
        add(nm)
    return off, n

COLOFF, NCOL = _col_layout()


def _colvec(v):
    return np.ascontiguousarray(np.asarray(v, np.float32).reshape(-1, 128).T)


def build_program(dbg=None):
    nc = bass.Bass("TRN2", target_bir_lowering=False)
    in_names = []
    def dt_in(name, shape, dt=F32):
        in_names.append(name)
        return nc.dram_tensor(name, list(shape), dt, kind="ExternalInput").ap()
    need_moe = dbg not in ("ada", "p2", "rwkv", "mix", "scan1")
    x_d = dt_in("x", [NT, D])
    cond_d = dt_in("cond", [128, 16])
    s0_d = dt_in("s0", [2, 32, 64, 64])
    keep_d = dt_in("keepc", [128, 2, NCH])
    tmask_d = dt_in("tmask", [128, 2, NT])
    poolMT_d = dt_in("poolMT", [4, NT, NT], BF16)
    poolic_d = dt_in("poolic", [128, 4, NT])
    cols_d = dt_in("cols", [128, NCOL])
    rbias_d = dt_in("rbias", [128, 2, NE])
    w_ada_d = dt_in("w_ada", [2, D, 6 * D])
    w_r_d = dt_in("rw_w_r", [D, D]); w_k_d = dt_in("rw_w_k", [D, D])
    w_v_d = dt_in("rw_w_v", [D, D]); w_o_d = dt_in("rw_w_o", [D, D])
    w1_d = dt_in("rw_w1", [2, D, 96]); w2_d = dt_in("rw_w2", [2, 96, D])
    a1_d = dt_in("rw_a1", [2, D, 96]); a2_d = dt_in("rw_a2", [2, 96, D])
    g1_d = dt_in("rw_g1", [D, 256]); g2_d = dt_in("rw_g2", [256, D])
    pool_w_d = dt_in("pool_w", [4, 512, 512])
    router_d = dt_in("moe_router", [2, D, NE])
    if need_moe:
        wg_d = dt_in("moe_w_gate", [2, NE, D, 512]); wu_d = dt_in("moe_w_up", [2, NE, D, 512])
        wd_d = dt_in("moe_w_down", [2, NE, 512, D])
    wsg_d = dt_in("moe_ws_gate", [2, D, 512]); wsu_d = dt_in("moe_ws_up", [2, D, 512])
    wsd_d = dt_in("moe_ws_down", [2, 512, D])
    y_d = nc.dram_tensor("y", [NT, D], F32, kind="ExternalOutput").ap()
    ns_d = nc.dram_tensor("ns", [4, 2, 32, 64, 64], F32, kind="ExternalOutput").ap()
    rkv_d = nc.dram_tensor("rkv_scratch", [3, NQ, 128, NT], F32, kind="Internal").ap()
    dbg_d = None
    if dbg is not None:
        dbg_d = nc.dram_tensor("dbg", [NQ, 128, NT], F32, kind="ExternalOutput").ap()

    k = KB(nc)
    V = lambda eng, fn, r=(), w=(): k.op(eng, fn, reads=r, writes=w)

    ident = k.sb([128, 128], F32, "ident")
    V("pool", lambda e: e.memset(ident[:], 0.0), w=[ident])
    V("pool", lambda e: e.affine_select(ident[:], ident[:], pattern=[[-1, 128]], compare_op=ALU.not_equal,
                                        fill=1.0, base=0, channel_multiplier=1), r=[ident], w=[ident])
    identb = k.sb([128, 128], BF16, "identb")
    V("dve", lambda e: e.tensor_copy(identb[:], ident[:]), r=[ident], w=[identb])
    ones_ln = k.sb([128, 128], F32, "ones_ln")
    V("pool", lambda e: e.memset(ones_ln[:], 1.0 / D), w=[ones_ln])
    blk1 = k.sb([128, 128], F32, "blk1")
    V("pool", lambda e: e.memset(blk1[:], 0.0), w=[blk1])
    V("pool", lambda e: e.memset(blk1[0:64, 0:64], 1.0), w=[blk1])
    V("pool", lambda e: e.memset(blk1[64:128, 64:128], 1.0), w=[blk1])
    blkm = k.sb([128, 128], F32, "blkm")
    V("dve", lambda e: e.tensor_scalar(blkm[:], blk1[:], 1.0 / 64, None, op0=ALU.mult), r=[blk1], w=[blkm])

    cols = k.sb([128, NCOL], F32, "cols")
    k.dma("sp", cols[:], cols_d, writes=[cols])
    C = lambda name, q=0, w=1: cols[:, COLOFF[name] + q: COLOFF[name] + q + w]

    def tri_mask(name, cmp_keep, zero_block, sgn=1):
        m = k.sb([128, 128], F32, name)
        V("pool", lambda e: e.memset(m[:], 1.0), w=[m])
        V("pool", lambda e: e.affine_select(m[:], m[:], pattern=[[sgn, 128]], compare_op=cmp_keep,
                                            fill=0.0, base=0, channel_multiplier=-sgn), r=[m], w=[m])
        if zero_block == "ur":
            V("pool", lambda e: e.memset(m[0:64, 64:128], 0.0), w=[m])
        else:
            V("pool", lambda e: e.memset(m[64:128, 0:64], 0.0), w=[m])
        return m
    UT_s = tri_mask("UTs", ALU.is_gt, "ur")
    UT_i = tri_mask("UTi", ALU.is_ge, "ur")
    LT_s = tri_mask("LTs", ALU.is_gt, "ll", -1)
    LT_i = tri_mask("LTi", ALU.is_ge, "ll", -1)
    MK1, MK2, MKA = [], [], []
    for d in range(2):
        sT, iT, sA = (UT_s, UT_i, LT_s) if d == 0 else (LT_s, LT_i, UT_s)
        m1 = k.sb([128, 256], F32, "MK1"); m2 = k.sb([128, 256], F32, "MK2"); ma = k.sb([128, 128], F32, "MKA")
        V("dve", lambda e: e.tensor_scalar(m1[:, 0:128], sT[:], -1.0, None, op0=ALU.mult), r=[sT], w=[m1])
        V("dve", lambda e: e.tensor_copy(m1[:, 128:256], iT[:]), r=[iT], w=[m1])
        V("dve", lambda e: e.tensor_copy(m2[:, 0:128], sT[:]), r=[sT], w=[m2])
        V("dve", lambda e: e.tensor_copy(m2[:, 128:256], iT[:]), r=[iT], w=[m2])
        V("dve", lambda e: e.tensor_scalar(ma[:], sA[:], -1.0, None, op0=ALU.mult), r=[sA], w=[ma])
        MK1.append(m1); MK2.append(m2); MKA.append(ma)
    bmask = k.sb([128, 2, CH], F32, "bmask")
    V("pool", lambda e: e.memset(bmask[:], 0.0), w=[bmask])
    V("pool", lambda e: e.memset(bmask[0:64, 0, :], 1.0), w=[bmask])
    V("pool", lambda e: e.memset(bmask[64:128, 1, :], 1.0), w=[bmask])
    cmask = k.sb([128, NCH, CH], F32, "cmask")
    V("pool", lambda e: e.memset(cmask[:], 1.0), w=[cmask])
    V("pool", lambda e: e.memset(cmask[:, :, 0:1], 0.0), w=[cmask])
    keepc = k.sb([128, 2, NCH], F32, "keepc")
    k.dma("sp", keepc[:], keep_d, writes=[keepc])
    tmask = k.sb([128, 2, NT], F32, "tmask")
    k.dma("sp", tmask[:], tmask_d, writes=[tmask])

    ada = k.sb([128, 2, 96], F32, "ada")
    opsc = k.sb([128, 2, 2, 16], F32, "opsc")
    with contextlib.ExitStack() as st:
        cond = k.sb([128, 16], F32, "cond", st)
        k.dma("sp", cond[:], cond_d, writes=[cond])
        scond = k.sb([128, 16], BF16, "scond", st)
        V("act", lambda e: e.activation(scond[:], cond[:], AF.Silu), r=[cond], w=[scond])
        wbuf = [k.sb([128, 16, 512], BF16, "adaw", st) for _ in range(3)]
        adap = k.ps([128, 2, 96], F32, "adap", st)
        n = 0
        for l in range(2):
            wv = w_ada_d[l].rearrange("(c p) n -> p c n", p=128)
            for pc in range(24):
                wb = wbuf[n % 3]; n += 1
                k.dma("pool", wb[:], wv[:, :, pc * 512:(pc + 1) * 512], writes=[wb])
                for j in range(4):
                    col = pc * 4 + j
                    for c in range(16):
                        V("pe", lambda e, wb=wb, j=j, c=c, col=col, l=l: e.matmul(
                            adap[:, l, col:col + 1], wb[:, c, j * 128:(j + 1) * 128], scond[:, c:c + 1],
                            start=(c == 0), stop=(c == 15)), r=[wb, scond], w=[adap])
            V("dve", lambda e, l=l: e.tensor_tensor(ada[:, l, :], adap[:, l, :], C("b_ada%d" % l, 0, 96), op=ALU.add),
              r=[adap, cols], w=[ada])
            for s in range(2):
                V("dve", lambda e, l=l, s=s: e.tensor_scalar(opsc[:, l, s, :], ada[:, l, (3 * s + 1) * 16:(3 * s + 2) * 16],
                                                          1.0, None, op0=ALU.add), r=[ada], w=[opsc])
        k.barrier()
    ADA = lambda l, part, q: ada[:, l, part * 16 + q: part * 16 + q + 1]
    OPSC = lambda l, s, q: opsc[:, l, s, q:q + 1]

    xT_all = k.sb([128, NQ, NT], F32, "xT")
    xT = [k.view(xT_all, "xT%d" % q) for q in range(NQ)]
    XT = lambda q: xT_all[:, q, :]
    bfA = k.sb([128, NQ, NT], BF16, "bfA")

    def load_xT(st):
        xtok = [k.sb([128, D], F32, "xtok", st) for _ in range(2)]
        tp = [k.ps([128, 4, 128], F32, "tp", st) for _ in range(2)]
        n = 0
        for tt in range(8):
            xb = xtok[tt % 2]
            k.dma("sp", xb[:], x_d[tt * 128:(tt + 1) * 128, :], writes=[xb])
            for qg in range(4):
                p = tp[n % 2]; n += 1
                for j in range(4):
                    q = qg * 4 + j
                    V("pe", lambda e, p=p, j=j, q=q, xb=xb: e.transpose(p[:, j, :], xb[:, q * 128:(q + 1) * 128], ident[:]),
                      r=[xb, ident], w=[p])
                eng = "act" if (n % 2) else "dve"
                if eng == "act":
                    V("act", lambda e, p=p, qg=qg, tt=tt: e.activation(xT_all[:, qg * 4:qg * 4 + 4, tt * 128:(tt + 1) * 128], p[:], AF.Copy),
                      r=[p], w=[xT[qg * 4 + j] for j in range(4)])
                else:
                    V("dve", lambda e, p=p, qg=qg, tt=tt: e.tensor_copy(xT_all[:, qg * 4:qg * 4 + 4, tt * 128:(tt + 1) * 128], p[:]),
                      r=[p], w=[xT[qg * 4 + j] for j in range(4)])

    def layer_norm(l, s, st):
        sq = [k.sb([128, 512], F32, "lnsq", st) for _ in range(2)]
        mean = k.sb([128, 512], F32, "lnmean", st)
        rstd = k.sb([128, 512], F32, "lnrstd", st)
        pm = k.ps([128, 512], F32, "lnpm", st)
        p2 = k.ps([128, 512], F32, "lnp2", st)
        for half in range(2):
            hs = slice(half * 512, (half + 1) * 512)
            for q in range(NQ):
                V("pe", lambda e, q=q: e.matmul(pm[:], ones_ln[:], xT_all[:, q, hs], start=(q == 0), stop=(q == NQ - 1)),
                  r=[ones_ln, xT[q]], w=[pm])
            for q in range(NQ):
                s_ = sq[q % 2]
                V("act", lambda e, q=q, s_=s_: e.activation(s_[:], xT_all[:, q, hs], AF.Square), r=[xT[q]], w=[s_])
                V("pe", lambda e, q=q, s_=s_: e.matmul(p2[:], ones_ln[:], s_[:], start=(q == 0), stop=(q == NQ - 1)),
                  r=[ones_ln, s_], w=[p2])
            V("act", lambda e: e.activation(mean[:], pm[:], AF.Copy), r=[pm], w=[mean])
            V("dve", lambda e: e.tensor_tensor(rstd[:], mean[:], mean[:], op=ALU.mult), r=[mean], w=[rstd])
            V("dve", lambda e: e.tensor_tensor(rstd[:], p2[:], rstd[:], op=ALU.subtract), r=[p2, rstd], w=[rstd])
            V("dve", lambda e: e.tensor_scalar(rstd[:], rstd[:], 0.0, LN_EPS, op0=ALU.max, op1=ALU.add), r=[rstd], w=[rstd])
            V("act", lambda e: e.activation(rstd[:], rstd[:], AF.Ln), r=[rstd], w=[rstd])
            V("act", lambda e: e.activation(rstd[:], rstd[:], AF.Exp, scale=-0.5), r=[rstd], w=[rstd])
            for q in range(NQ):
                eng = "dve" if q % 2 == 0 else "pool"
                V(eng, lambda e, q=q: e.tensor_tensor(xT_all[:, q, hs], xT_all[:, q, hs], mean[:], op=ALU.subtract),
                  r=[xT[q], mean], w=[xT[q]])
                V(eng, lambda e, q=q: e.tensor_tensor(xT_all[:, q, hs], xT_all[:, q, hs], rstd[:], op=ALU.mult),
                  r=[xT[q], rstd], w=[xT[q]])
                V(eng, lambda e, q=q: e.tensor_scalar(xT_all[:, q, hs], xT_all[:, q, hs], C("ln_g%d%d" % (l, s), q),
                                                      C("ln_b%d%d" % (l, s), q), op0=ALU.mult, op1=ALU.add),
                  r=[xT[q], cols], w=[xT[q]])

    def moe(l, st0):
        st = contextlib.ExitStack()
        h2_all = bfA
        h2 = [k.view(h2_all, "h2_%d" % q) for q in range(NQ)]
        gT = k.sb([64, NT], F32, "gT", st)
        with contextlib.ExitStack() as s1:
            wr = k.sb([128, NQ, NE], F32, "wr", s1)
            k.dma("sp", wr[:], router_d[l].rearrange("(c p) n -> p c n", p=128), writes=[wr])
            rb = k.sb([128, NE], F32, "rb", s1)
            k.dma("sp", rb[:], rbias_d[:, l, :], writes=[rb])
            hf = [k.sb([128, NT], F32, "h2f", s1) for _ in range(2)]
            s1a = contextlib.ExitStack()
            lgs = [k.ps([128, 512], F32, "lg", s1a) for _ in range(8)]
            for q in range(NQ):
                h_ = hf[q % 2]
                V("dve", lambda e: e.tensor_scalar(h_[:], XT(q), OPSC(l, 1, q), ADA(l, 3, q), op0=ALU.mult, op1=ALU.add),
                  r=[xT[q], opsc, ada], w=[h_])
                V("act", lambda e: e.activation(h2_all[:, q, :], h_[:], AF.Copy), r=[h_], w=[h2[q]])
                for tt in range(8):
                    V("pe", lambda e: e.matmul(lgs[tt][:, 0:NE], h_[:, tt * 128:(tt + 1) * 128], wr[:, q, :],
                                               start=(q == 0), stop=(q == NQ - 1)),
                      r=[h_, wr], w=[lgs[tt]])
                V("pool", lambda e: e.tensor_scalar(XT(q), XT(q), ALPHA, None, op0=ALU.mult), r=[xT[q]], w=[xT[q]])
            sc = k.sb([128, 8, NE], F32, "sc", s1)
            for tt in range(8):
                V("act", lambda e: e.activation(sc[:, tt, :], lgs[tt][:, 0:NE], AF.Sigmoid), r=[lgs[tt]], w=[sc])
            k.barrier()
            s1a.close()
            bi = k.sb([128, 8, NE], F32, "bi", s1)
            V("dve", lambda e: e.tensor_tensor(bi[:], sc[:], rb[:].unsqueeze(1).broadcast_to([128, 8, NE]), op=ALU.add),
              r=[sc, rb], w=[bi])
            m1 = k.sb([128, 64], F32, "m1", s1); m2 = k.sb([128, 64], F32, "m2", s1)
            tmp = k.sb([128, 8, NE], F32, "rtmp", s1)
            bi3 = bi[:].rearrange("p a (g e) -> p (a g) e", e=8)
            tmp3 = tmp[:].rearrange("p a (g e) -> p (a g) e", e=8)
            V("dve", lambda e: e.tensor_reduce(m1[:], bi3, axis=AX.X, op=ALU.max), r=[bi], w=[m1])
            V("dve", lambda e: e.tensor_tensor(tmp3, bi3, m1[:].unsqueeze(2).broadcast_to([128, 64, 8]), op=ALU.is_equal),
              r=[bi, m1], w=[tmp])
            V("dve", lambda e: e.scalar_tensor_tensor(tmp[:], tmp[:], -1e30, bi[:], op0=ALU.mult, op1=ALU.add), r=[tmp, bi], w=[tmp])
            V("dve", lambda e: e.tensor_reduce(m2[:], tmp3, axis=AX.X, op=ALU.max), r=[tmp], w=[m2])
            V("dve", lambda e: e.tensor_tensor(m1[:], m1[:], m2[:], op=ALU.add), r=[m1, m2], w=[m1])
            srt = k.sb([128, 8, 8], F32, "srt", s1)
            for tt in range(8):
                V("dve", lambda e: e.max(srt[:, tt, :], m1[:, tt * 8:(tt + 1) * 8]), r=[m1], w=[srt])
            gm = k.sb([128, 64], F32, "gm", s1)
            V("dve", lambda e: e.tensor_tensor(gm[:].rearrange("p (a g) -> p a g", g=8), m1[:].rearrange("p (a g) -> p a g", g=8),
                                               srt[:, :, 3:4].broadcast_to([128, 8, 8]), op=ALU.is_ge), r=[m1, srt], w=[gm])
            V("dve", lambda e: e.tensor_scalar(gm[:], gm[:], -1.0, 1e30, op0=ALU.add, op1=ALU.mult), r=[gm], w=[gm])
            V("dve", lambda e: e.tensor_tensor(tmp3, bi3, gm[:].unsqueeze(2).broadcast_to([128, 64, 8]), op=ALU.add),
              r=[bi, gm], w=[tmp])
            for tt in range(8):
                V("dve", lambda e: e.max(srt[:, tt, :], tmp[:, tt, :]), r=[tmp], w=[srt])
            sel = k.sb([128, 8, NE], F32, "sel", s1)
            V("dve", lambda e: e.tensor_tensor(sel[:], tmp[:], srt[:, :, 5:6].broadcast_to([128, 8, NE]), op=ALU.is_ge),
              r=[tmp, srt], w=[sel])
            V("dve", lambda e: e.tensor_tensor(sel[:], sel[:], sc[:], op=ALU.mult), r=[sel, sc], w=[sel])
            den = k.sb([128, 8], F32, "den", s1)
            V("dve", lambda e: e.tensor_reduce(den[:], sel[:], axis=AX.X, op=ALU.add), r=[sel], w=[den])
            V("dve", lambda e: e.tensor_scalar(den[:], den[:], 1e-20, None, op0=ALU.add), r=[den], w=[den])
            V("dve", lambda e: e.reciprocal(den[:], den[:]), r=[den], w=[den])
            V("dve", lambda e: e.tensor_scalar(den[:], den[:], 2.5, None, op0=ALU.mult), r=[den], w=[den])
            V("dve", lambda e: e.tensor_tensor(sel[:], sel[:], den[:].unsqueeze(2).broadcast_to([128, 8, NE]), op=ALU.mult),
              r=[sel, den], w=[sel])
            gtp = k.ps([64, NT], F32, "gtp", s1)
            for tt in range(8):
                V("pe", lambda e: e.transpose(gtp[:, tt * 128:(tt + 1) * 128], sel[:, tt, :], ident[:]), r=[sel, ident], w=[gtp])
            V("act", lambda e: e.activation(gT[:], gtp[:], AF.Copy), r=[gtp], w=[gT])
            k.barrier()
        gu = [k.sb([128, 16, 256], BF16, "gu", st) for _ in range(4)]
        dd = [k.sb([128, 2, D], BF16, "dd", st) for _ in range(2)]
        sgb = [k.sb([128, 512], F32, "sg", st) for _ in range(2)]
        t1b = [k.sb([128, 512], F32, "t1", st) for _ in range(2)]
        hact_all = [k.sb([128, 4, NT], BF16, "hact", st) for _ in range(2)]
        gbc = [k.sb([128, NT], F32, "gbc", st) for _ in range(2)]
        g_ps = [k.ps([128, 512], F32, "gps", st) for _ in range(2)]
        u_ps = [k.ps([128, 512], F32, "ups", st) for _ in range(2)]
        d_ps = [k.ps([128, 512], F32, "dps", st) for _ in range(2)]
        b_ps = [k.ps([128, 512], F32, "bps", st) for _ in range(1)]
        cnt = {"g": 0, "d": 0}
        for ei in range(-1, NE):
            if ei < 0:
                wgv = wsg_d[l].rearrange("(c p) n -> p c n", p=128)
                wuv = wsu_d[l].rearrange("(c p) n -> p c n", p=128)
                wdv = wsd_d[l].rearrange("(c p) n -> p c n", p=128)
            else:
                wgv = wg_d[l, ei].rearrange("(c p) n -> p c n", p=128)
                wuv = wu_d[l, ei].rearrange("(c p) n -> p c n", p=128)
                wdv = wd_d[l, ei].rearrange("(c p) n -> p c n", p=128)
            for hh in range(2):
                k.dma("pool", gu[2 * hh][:], wgv[:, :, hh * 256:(hh + 1) * 256], writes=[gu[2 * hh]])
                k.dma("pool", gu[2 * hh + 1][:], wuv[:, :, hh * 256:(hh + 1) * 256], writes=[gu[2 * hh + 1]])
            for hh in range(2):
                k.dma("pool", dd[hh][:], wdv[:, 2 * hh:2 * hh + 2, :], writes=[dd[hh]])
            ha = hact_all[(ei + 1) % 2]
            gb = gbc[(ei + 1) % 2]
            if ei >= 0:
                for half in range(2):
                    bp = b_ps[0]
                    V("pe", lambda e: e.matmul(bp[:], ident[0:64, ei:ei + 1].broadcast_to([64, 128]),
                                               gT[:, half * 512:(half + 1) * 512], start=True, stop=True), r=[ident, gT], w=[bp])
                    V("act", lambda e: e.activation(gb[:, half * 512:(half + 1) * 512], bp[:], AF.Copy), r=[bp], w=[gb])
            for j in range(4):
                wgp = gu[2 * (j // 2)]; wup = gu[2 * (j // 2) + 1]; jc = (j % 2) * 128
                for half in range(2):
                    hs = slice(half * 512, (half + 1) * 512)
                    gp = g_ps[cnt["g"] % 2]; up = u_ps[cnt["g"] % 2]
                    sg = sgb[cnt["g"] % 2]; t1 = t1b[cnt["g"] % 2]; cnt["g"] += 1
                    for c in range(NQ):
                        V("pe", lambda e: e.matmul(gp[:], wgp[:, c, jc:jc + 128], h2_all[:, c, hs], start=(c == 0), stop=(c == NQ - 1)),
                          r=[wgp, h2[c]], w=[gp])
                    for c in range(NQ):
                        V("pe", lambda e: e.matmul(up[:], wup[:, c, jc:jc + 128], h2_all[:, c, hs], start=(c == 0), stop=(c == NQ - 1)),
                          r=[wup, h2[c]], w=[up])
                    V("act", lambda e: e.activation(sg[:], gp[:], AF.Silu), r=[gp], w=[sg])
                    if ei >= 0:
                        V("dve", lambda e: e.tensor_tensor(t1[:], up[:], sg[:], op=ALU.mult), r=[up, sg], w=[t1])
                        V("dve", lambda e: e.tensor_tensor(ha[:, j, hs], t1[:], gb[:, hs], op=ALU.mult), r=[t1, gb], w=[ha])
                    else:
                        V("dve", lambda e: e.tensor_tensor(ha[:, j, hs], up[:], sg[:], op=ALU.mult), r=[up, sg], w=[ha])
            for f in range(NQ):
                for half in range(2):
                    hs = slice(half * 512, (half + 1) * 512)
                    dp = d_ps[cnt["d"] % 2]; cnt["d"] += 1
                    for j in range(4):
                        V("pe", lambda e: e.matmul(dp[:], dd[j // 2][:, j % 2, f * 128:(f + 1) * 128], ha[:, j, hs],
                                                   start=(j == 0), stop=(j == 3)), r=[dd[j // 2], ha], w=[dp])
                    V("dve", lambda e: e.scalar_tensor_tensor(xT_all[:, f, hs], dp[:], ADA(l, 5, f), xT_all[:, f, hs],
                                                              op0=ALU.mult, op1=ALU.add), r=[dp, ada, xT[f]], w=[xT[f]])
        k.barrier()
        st.close()

    def rwkv_layer(l=0):
        st = contextlib.ExitStack()
        with contextlib.ExitStack() as s0_:
            load_xT(s0_)
            k.barrier()
        tw = [k.sb([96, NT], BF16, "tw", st) for _ in range(2)]
        ta = [k.sb([96, NT], BF16, "ta", st) for _ in range(2)]
        sgl = k.sb([128, 2, NT], BF16, "sgl", st)
        negw0 = k.sb([128, 2, 16], F32, "negw0", st)
        omka = k.sb([128, 16], F32, "omka", st)
        for d in range(2):
            V("dve", lambda e: e.tensor_scalar(negw0[:, d, :], C("w0%d" % d, 0, 16), -1.0, None, op0=ALU.mult), r=[cols], w=[negw0])
        V("dve", lambda e: e.tensor_scalar(omka[:], C("k_a", 0, 16), -1.0, 1.0, op0=ALU.mult, op1=ALU.add), r=[cols], w=[omka])

        def mixes(q, idxs, outs, hp, dp, dn, tmpx):
            V("dve", lambda e: e.tensor_scalar(hp[:, 1:NT + 1], XT(q), OPSC(l, 0, q), ADA(l, 0, q), op0=ALU.mult, op1=ALU.add),
              r=[xT[q], opsc, ada], w=[hp])
            V("pool", lambda e: e.tensor_tensor(dp[:], hp[:, 0:NT], tmask[:, 0, :], op=ALU.mult), r=[hp, tmask], w=[dp])
            V("pool", lambda e: e.tensor_tensor(dp[:], dp[:], hp[:, 1:NT + 1], op=ALU.subtract), r=[dp, hp], w=[dp])
            V("dve", lambda e: e.tensor_tensor(dn[:], hp[:, 2:NT + 2], tmask[:, 1, :], op=ALU.mult), r=[hp, tmask], w=[dn])
            V("dve", lambda e: e.tensor_tensor(dn[:], dn[:], hp[:, 1:NT + 1], op=ALU.subtract), r=[dn, hp], w=[dn])
            for n_, (i, (ot, oap)) in enumerate(zip(idxs, outs)):
                eng = "dve"
                V(eng, lambda e: e.scalar_tensor_tensor(tmpx[:], dp[:], C("mp%d" % i, q), hp[:, 1:NT + 1], op0=ALU.mult, op1=ALU.add),
                  r=[dp, hp, cols], w=[tmpx])
                V(eng, lambda e: e.scalar_tensor_tensor(oap, dn[:], C("mn%d" % i, q), tmpx[:], op0=ALU.mult, op1=ALU.add),
                  r=[dn, tmpx, cols], w=[ot])

        with contextlib.ExitStack() as s1:
            hp = k.sb([128, NT + 2], F32, "hp", s1)
            V("pool", lambda e: e.memset(hp[:], 0.0), w=[hp])
            dp = k.sb([128, NT], F32, "dp", s1); dn = k.sb([128, NT], F32, "dn", s1); tmpx = k.sb([128, NT], F32, "tmpx", s1)
            if dbg == "mix":
                xo = k.sb([128, NT], BF16, "xo", s1)
                mixes(3, [0], [(xo, xo[:])], hp, dp, dn, tmpx)
                k.dma("sp", dbg_d[0], hp[:, 1:NT + 1], reads=[hp])
                k.dma("sp", dbg_d[1], dp[:], reads=[dp])
                k.dma("sp", dbg_d[2], dn[:], reads=[dn])
                k.dma("sp", dbg_d[3], tmpx[:], reads=[tmpx])
                xf = k.sb([128, NT], F32, "xf", s1)
                V("dve", lambda e: e.tensor_copy(xf[:], xo[:]), r=[xo], w=[xf])
                k.dma("sp", dbg_d[4], xf[:], reads=[xf])
                k.dma("sp", dbg_d[5], tmask[:, 0, :], reads=[tmask])
                k.dma("sp", dbg_d[6], tmask[:, 1, :], reads=[tmask])
                k.barrier()
                return st, None, None, None, None, None
            with contextlib.ExitStack() as s2:
                w1s = k.sb([128, NQ, 2, 96], BF16, "w1s", s2); a1s = k.sb([128, NQ, 2, 96], BF16, "a1s", s2)
                g1s = k.sb([128, NQ, 256], BF16, "g1s", s2)
                for d in range(2):
                    k.dma("pool", w1s[:, :, d, :], w1_d[d].rearrange("(c p) n -> p c n", p=128), writes=[w1s])
                    k.dma("pool", a1s[:, :, d, :], a1_d[d].rearrange("(c p) n -> p c n", p=128), writes=[a1s])
                k.dma("pool", g1s[:], g1_d.rearrange("(c p) n -> p c n", p=128), writes=[g1s])
                xs2 = [k.sb([128, NT], BF16, "xs2", s2) for _ in range(4)]
                pp = [k.ps([128, 512], F32, "lp", s2) for _ in range(8)]
                for q in range(NQ):
                    xw = xs2[(q % 2) * 2]; xg = xs2[(q % 2) * 2 + 1]
                    mixes(q, [1, 5], [(xw, xw[:]), (xg, xg[:])], hp, dp, dn, tmpx)
                    for d in range(2):
                        for half in range(2):
                            V("pe", lambda e: e.matmul(pp[d * 2 + half][0:96, :], w1s[:, q, d, :], xw[:, half * 512:(half + 1) * 512],
                                                       start=(q == 0), stop=(q == NQ - 1)), r=[w1s, xw], w=[pp[d * 2 + half]])
                    for j in range(2):
                        for half in range(2):
                            V("pe", lambda e: e.matmul(pp[4 + j * 2 + half][:], g1s[:, q, j * 128:(j + 1) * 128], xg[:, half * 512:(half + 1) * 512],
                                                       start=(q == 0), stop=(q == NQ - 1)), r=[g1s, xg], w=[pp[4 + j * 2 + half]])
                for d in range(2):
                    for half in range(2):
                        V("act", lambda e: e.activation(tw[d][:, half * 512:(half + 1) * 512], pp[d * 2 + half][0:96, :], AF.Tanh),
                          r=[pp[d * 2 + half]], w=[tw[d]])
                for j in range(2):
                    for half in range(2):
                        V("act", lambda e: e.activation(sgl[:, j, half * 512:(half + 1) * 512], pp[4 + j * 2 + half][:], AF.Sigmoid),
                          r=[pp[4 + j * 2 + half]], w=[sgl])
                for q in range(NQ):
                    xa = xs2[q % 4]
                    mixes(q, [4], [(xa, xa[:])], hp, dp, dn, tmpx)
                    for d in range(2):
                        for half in range(2):
                            V("pe", lambda e: e.matmul(pp[d * 2 + half][0:96, :], a1s[:, q, d, :], xa[:, half * 512:(half + 1) * 512],
                                                       start=(q == 0), stop=(q == NQ - 1)), r=[a1s, xa], w=[pp[d * 2 + half]])
                for d in range(2):
                    for half in range(2):
                        V("act", lambda e: e.activation(ta[d][:, half * 512:(half + 1) * 512], pp[d * 2 + half][0:96, :], AF.Copy),
                          r=[pp[d * 2 + half]], w=[ta[d]])
                k.barrier()
            with contextlib.ExitStack() as s2:
                xs_all = bfA
                xsv = [k.view(xs_all, "xs%d" % q) for q in range(NQ)]
                wpc = [k.sb([128, NQ, 512], BF16, "wpc", s2) for _ in range(2)]
                stg = [k.sb([128, NT], F32, "stg", s2) for _ in range(2)]
                pj = [k.ps([128, 512], F32, "pj", s2) for _ in range(4)]
                npc = 0; nst = 0; npj = 0
                for p_, (mi, wdram) in enumerate(((0, w_r_d), (2, w_k_d), (3, w_v_d))):
                    for q in range(NQ):
                        mixes(q, [mi], [(xsv[q], xs_all[:, q, :])], hp, dp, dn, tmpx)
                    wv = wdram.rearrange("(c p) n -> p c n", p=128)
                    for n_ in range(4):
                        wb = wpc[npc % 2]; npc += 1
                        k.dma("pool", wb[:], wv[:, :, n_ * 512:(n_ + 1) * 512], writes=[wb])
                        for jj in range(4):
                            qo = n_ * 4 + jj
                            sg_ = stg[nst % 2]; nst += 1
                            for half in range(2):
                                pz = pj[npj % 4]; npj += 1
                                for c in range(NQ):
                                    V("pe", lambda e: e.matmul(pz[:], wb[:, c, jj * 128:(jj + 1) * 128], xs_all[:, c, half * 512:(half + 1) * 512],
                                                               start=(c == 0), stop=(c == NQ - 1)), r=[wb, xsv[c]], w=[pz])
                                if half == 0:
                                    V("act", lambda e: e.activation(sg_[:, 0:512], pz[:], AF.Copy), r=[pz], w=[sg_])
                                else:
                                    V("dve", lambda e: e.tensor_copy(sg_[:, 512:1024], pz[:]), r=[pz], w=[sg_])
                            k.dma("sp", rkv_d[p_, qo], sg_[:], reads=[sg_])
                k.barrier()
        return st, tw, ta, sgl, negw0, omka

    def rwkv_scan(l, st, tw, ta, sgl, negw0, omka):
        z_all = bfA
        zv = [k.view(z_all, "z%d" % q) for q in range(NQ)]
        s3 = contextlib.ExitStack()
        F = lambda nm: k.sb([128, NT], F32, nm, s3)
        XV = lambda i, nm: TV(xT_all[:, i, :], nm)
        XV2 = lambda i, nm: TV(xT_all[:, i:i + 2, :], nm)
        KtRt = [XV2(0, "KtRt0"), XV2(2, "KtRt1")]
        AhKh = [XV2(4, "AhKh0"), XV2(6, "AhKh1")]
        r_, kr, v_ = XV(8, "r_"), XV(9, "kr"), XV(10, "v_")
        g_q, kap = XV(11, "g_q"), XV(12, "kap")
        lw, a_, cum = XV(13, "lw"), XV(14, "a_"), XV(15, "cum")
        Ep, Em, Ex, t0 = F("Ep"), F("Em"), F("Ex"), F("t0")
        yd = [F("y0"), F("y1")]
        glast = [k.sb([128, NCH], F32, "glast", s3) for _ in range(2)]
        w2s = k.sb([96, 2, 128], BF16, "w2s", s3); a2s = k.sb([96, 2, 128], BF16, "a2s", s3)
        g2s = k.sb([128, 2, 128], BF16, "g2s", s3)
        pbig = [k.ps([128, 512], F32, "pbig", s3) for _ in range(2)]
        pbon = [k.ps([128, 512], F32, "pbon", s3) for _ in range(2)]
        ps12 = [k.ps([128, 512], F32, "ps12", s3) for _ in range(2)]
        ptok = [k.ps([128, 512], F32, "ptok", s3) for _ in range(1)]
        psm_h = [k.ps([128, 512], F32, "psm", s3) for _ in range(1)]
        psm = [pbig[0], pbig[1], pbon[0], pbon[1], psm_h[0]]
        ring = {"psm": 0, "sb": 0, "big": 0}
        sbr = [k.sb([128, 128], F32, "sbr", s3) for _ in range(40)]
        def SB():
            t = sbr[ring["sb"] % len(sbr)]; ring["sb"] += 1; return t
        def PS():
            t = psm[ring["psm"] % len(psm)]; ring["psm"] += 1; return t, t.h[:, 0:128]
        def PB():
            t = pbig[ring["big"] % 2]; ring["big"] += 1; return t
        KR = [[k.sb([128, 2, 2, CH], F32, "KR", s3) for _ in range(2)] for _ in range(2)]
        AK = [[k.sb([128, 2, 2, CH], F32, "AK", s3) for _ in range(2)] for _ in range(2)]
        Vb = [k.sb([128, 2, CH], F32, "Vb", s3) for _ in range(2)]
        tokv = [k.sb([128, 128], F32, "tokv", s3) for _ in range(2)]
        tokak = [[k.sb([128, 2, 128], F32, "tokak", s3) for _ in range(2)] for _ in range(2)]
        SC1 = [[k.sb([128, 256], F32, "SC1", s3) for _ in range(2)] for _ in range(2)]
        SC2 = [[k.sb([128, 256], F32, "SC2", s3) for _ in range(2)] for _ in range(2)]
        Pst = [[k.sb([128, 128], F32, "P", s3) for _ in range(3)] for _ in range(2)]
        sbd = [k.sb([128, 128], F32, "sbd", s3) for _ in range(2)]
        snap = [k.sb([128, 128], F32, "snap", s3) for _ in range(4)]
        nsnap = [0]
        for d in range(2):
            V("pool", lambda e: e.memset(sbd[d][:], 0.0), w=[sbd[d]])
        H = lambda half: slice(half * 512, (half + 1) * 512)

        for q in range(NQ if dbg != "scan1" else 1):
            qs = slice(q * 128, (q + 1) * 128)
            k.dma("sp", r_[:], rkv_d[0, q], writes=[r_])
            k.dma("sp", kr[:], rkv_d[1, q], writes=[kr])
            k.dma("sp", v_[:], rkv_d[2, q], writes=[v_])
            k.dma("pool", w2s[:], w2_d.rearrange("d k n -> k d n")[:, :, qs], writes=[w2s])
            k.dma("pool", a2s[:], a2_d.rearrange("d k n -> k d n")[:, :, qs], writes=[a2s])
            k.dma("pool", g2s[:], g2_d.rearrange("(c p) n -> p c n", p=128)[:, :, qs], writes=[g2s])
            Pcur = [None, None]
            for d in range(2):
                for hh in range(2):
                    k.dma("sp", sbd[d][hh * 64:(hh + 1) * 64, hh * 64:(hh + 1) * 64], s0_d[d, 2 * q + hh], writes=[sbd[d]])
                pt_, pap = PS()
                V("pe", lambda e: e.transpose(pap, sbd[d][:], ident[:]), r=[sbd[d], ident], w=[pt_])
                Pcur[d] = Pst[d][0]
                V("act", lambda e: e.activation(Pcur[d][:], pap, AF.Copy), r=[pt_], w=[Pcur[d]])
            for half in range(2):
                pb = PB()
                for c in range(2):
                    V("pe", lambda e: e.matmul(pb[:], g2s[:, c, :], sgl[:, c, H(half)], start=(c == 0), stop=(c == 1)), r=[g2s, sgl], w=[pb])
                V("act", lambda e: e.activation(g_q[:, H(half)], pb[:], AF.Copy), r=[pb], w=[g_q])
            V("dve", lambda e: e.tensor_scalar(kap[:], kr[:], C("k_k", q), None, op0=ALU.mult), r=[kr, cols], w=[kap])
            V("pool", lambda e: e.tensor_tensor(t0[:], kap[:], kap[:], op=ALU.mult), r=[kap], w=[t0])
            for half in range(2):
                pb = PB()
                V("pe", lambda e: e.matmul(pb[:], blk1[:], t0[:, H(half)], start=True, stop=True), r=[blk1, t0], w=[pb])
                V("dve", lambda e: e.tensor_scalar(Ex[:, H(half)], pb[:], 1e-24, None, op0=ALU.max), r=[pb], w=[Ex])
                V("act", lambda e: e.activation(Ex[:, H(half)], Ex[:, H(half)], AF.Ln), r=[Ex], w=[Ex])
                V("act", lambda e: e.activation(Ex[:, H(half)], Ex[:, H(half)], AF.Exp, scale=-0.5), r=[Ex], w=[Ex])
            V("dve", lambda e: e.tensor_tensor(kap[:], kap[:], Ex[:], op=ALU.mult), r=[kap, Ex], w=[kap])
            for d in range(2):
                for half in range(2):
                    pb = PB()
                    V("pe", lambda e: e.matmul(pb[:], w2s[:, d, :], tw[d][:, H(half)], start=True, stop=True), r=[w2s, tw[d]], w=[pb])
                    V("act", lambda e: e.activation(lw[:, H(half)], pb[:], AF.Exp, bias=negw0[:, d, q:q + 1], scale=-1.0), r=[pb, negw0], w=[lw])
                    pb2 = PB()
                    V("pe", lambda e: e.matmul(pb2[:], a2s[:, d, :], ta[d][:, H(half)], start=True, stop=True), r=[a2s, ta[d]], w=[pb2])
                    V("act", lambda e: e.activation(a_[:, H(half)], pb2[:], AF.Sigmoid, bias=C("a0%d" % d, q), scale=1.0), r=[pb2, cols], w=[a_])
                V("dve", lambda e: e.tensor_scalar(lw[:], lw[:], 1.0, None, op0=ALU.add), r=[lw], w=[lw])
                V("act", lambda e: e.activation(lw[:], lw[:], AF.Ln), r=[lw], w=[lw])
                V("dve", lambda e: e.tensor_scalar(lw[:], lw[:], -1.0, -0.5, op0=ALU.mult, op1=ALU.add), r=[lw], w=[lw])
                V("act", lambda e: e.activation(lw[:], lw[:], AF.Exp), r=[lw], w=[lw])
                V("dve", lambda e: e.tensor_scalar(lw[:], lw[:], -1.0, None, op0=ALU.mult), r=[lw], w=[lw])
                V("dve", lambda e: e.tensor_scalar(t0[:], a_[:], C("k_a", q), omka[:, q:q + 1], op0=ALU.mult, op1=ALU.add), r=[a_, cols, omka], w=[t0])
                V("dve", lambda e: e.tensor_tensor(AhKh[d][:, 1, :], t0[:], kr[:], op=ALU.mult), r=[t0, kr], w=[AhKh[d]])
                V("pool", lambda e: e.tensor_tensor(AhKh[d][:, 0, :], kap[:], a_[:], op=ALU.mult), r=[kap, a_], w=[AhKh[d]])
                V("pool", lambda e: e.tensor_tensor(t0[:], r_[:], AhKh[d][:, 1, :], op=ALU.mult), r=[r_, AhKh[d], t0], w=[t0])
                V("pool", lambda e: e.tensor_scalar(t0[:], t0[:], C("r_k", q), None, op0=ALU.mult), r=[t0, cols], w=[t0])
                for half in range(2):
                    V("pe", lambda e: e.matmul(pbon[half][:], blk1[:], t0[:, H(half)], start=(d == 0), stop=(d == 1)), r=[blk1, t0], w=[pbon[half]])
                V("dve", lambda e: e.tensor_tensor_scan(cum[:], cmask[:].rearrange("p c t -> p (c t)"), lw[:], 0.0, op0=ALU.mult, op1=ALU.add),
                  r=[cmask, lw], w=[cum])
                if d == 1:
                    c3 = cum[:].rearrange("p (c t) -> p c t", t=CH)
                    V("dve", lambda e: e.tensor_copy(glast[d][:], c3[:, :, CH - 1]), r=[cum], w=[glast[d]])
                    V("dve", lambda e: e.tensor_tensor(cum[:], lw[:], cum[:], op=ALU.subtract), r=[lw, cum], w=[cum])
                    V("dve", lambda e: e.tensor_tensor(c3, c3, glast[d][:].unsqueeze(2).broadcast_to([128, NCH, CH]), op=ALU.add),
                      r=[cum, glast[d]], w=[cum])
                V("act", lambda e: e.activation(Ep[:], cum[:], AF.Exp), r=[cum], w=[Ep])
                V("act", lambda e: e.activation(Em[:], cum[:], AF.Exp, scale=-1.0), r=[cum], w=[Em])
                V("pool", lambda e: e.tensor_tensor(t0[:], cum[:], lw[:], op=ALU.subtract), r=[cum, lw], w=[t0])
                V("act", lambda e: e.activation(Ex[:], t0[:], AF.Exp), r=[t0], w=[Ex])
                e3 = Ep[:].rearrange("p (c t) -> p c t", t=CH)
                V("dve", lambda e: e.tensor_copy(glast[d][:], e3[:, :, CH - 1] if d == 0 else e3[:, :, 0]), r=[Ep], w=[glast[d]])
                V("dve", lambda e: e.tensor_tensor(KtRt[d][:, 0, :], kap[:], Ex[:], op=ALU.mult), r=[kap, Ex], w=[KtRt[d]])
                V("pool", lambda e: e.tensor_tensor(KtRt[d][:, 1, :], r_[:], Ep[:], op=ALU.mult), r=[r_, Ep], w=[KtRt[d]])
                V("dve", lambda e: e.tensor_tensor(AhKh[d][:, 0, :], AhKh[d][:, 0, :], Em[:], op=ALU.mult), r=[AhKh[d], Em], w=[AhKh[d]])
                V("pool", lambda e: e.tensor_tensor(AhKh[d][:, 1, :], AhKh[d][:, 1, :], Em[:], op=ALU.mult), r=[AhKh[d], Em], w=[AhKh[d]])
            for half in range(2):
                V("dve", lambda e: e.tensor_tensor(Ex[:, H(half)], pbon[half][:], v_[:, H(half)], op=ALU.mult), r=[pbon[half], v_], w=[Ex])
            bm4 = bmask[:].unsqueeze(1).broadcast_to([128, 2, 2, CH])
            def unit(step, d):
                c = step if d == 0 else NCH - 1 - step
                cs = slice(c * CH, (c + 1) * CH)
                kr_t = KR[d][step % 2]; ak_t = AK[d][step % 2]
                V("pool", lambda e: e.tensor_tensor(kr_t[:], KtRt[d][:, :, cs].unsqueeze(2).broadcast_to([128, 2, 2, CH]), bm4, op=ALU.mult),
                  r=[KtRt[d], bmask], w=[kr_t])
                V("pool", lambda e: e.tensor_tensor(ak_t[:], AhKh[d][:, :, cs].unsqueeze(2).broadcast_to([128, 2, 2, CH]), bm4, op=ALU.mult),
                  r=[AhKh[d], bmask], w=[ak_t])
                vb = Vb[d]; tv = tokv[d]
                V("pool", lambda e: e.tensor_tensor(vb[:], v_[:, cs].unsqueeze(1).broadcast_to([128, 2, CH]), bmask[:], op=ALU.mult),
                  r=[v_, bmask], w=[vb])
                KRf = lambda i: kr_t[:, i, :, :].rearrange("p a t -> p (a t)")
                AKf = lambda i: ak_t[:, i, :, :].rearrange("p a t -> p (a t)")
                KR2 = kr_t[:].rearrange("p i a t -> p (i a t)")
                vbf = vb[:].rearrange("p a t -> p (a t)")
                pk = ptok[0]
                V("pe", lambda e: e.transpose(pk[:, 0:128], AKf(0), ident[:]), r=[ak_t, ident], w=[pk])
                V("pe", lambda e: e.transpose(pk[:, 128:256], AKf(1), ident[:]), r=[ak_t, ident], w=[pk])
                V("pe", lambda e: e.transpose(pk[:, 256:384], vbf, ident[:]), r=[vb, ident], w=[pk])
                tak = tokak[d][step % 2]
                V("act", lambda e: e.activation(tak[:].rearrange("p i t -> p (i t)"), pk[:, 0:256], AF.Copy), r=[pk], w=[tak])
                V("act", lambda e: e.activation(tv[:], pk[:, 256:384], AF.Copy), r=[pk], w=[tv])
                yield
                p12 = ps12[d]
                V("pe", lambda e: e.matmul(p12[:, 0:256], AKf(0), KR2, start=True, stop=True), r=[ak_t, kr_t], w=[p12])
                V("pe", lambda e: e.matmul(p12[:, 256:512], AKf(1), KR2, start=True, stop=True), r=[ak_t, kr_t], w=[p12])
                p3t, p3 = PS()
                V("pe", lambda e: e.matmul(p3, KRf(0), AKf(0), start=True, stop=True), r=[kr_t, ak_t], w=[p3t])
                sc1 = SC1[d][step % 2]; sc2 = SC2[d][step % 2]
                V("dve", lambda e: e.tensor_tensor(sc1[:], p12[:, 0:256], MK1[d][:], op=ALU.mult), r=[p12, MK1[d]], w=[sc1])
                V("dve", lambda e: e.tensor_tensor(sc2[:], p12[:, 256:512], MK2[d][:], op=ALU.mult), r=[p12, MK2[d]], w=[sc2])
                X = SB()
                V("dve", lambda e: e.tensor_tensor(X[:], p3, MKA[d][:], op=ALU.mult), r=[p3t, MKA[d]], w=[X])
                TT = SB()
                V("pool", lambda e: e.tensor_tensor(TT[:], sc1[:, 0:128], ident[:], op=ALU.add), r=[sc1, ident], w=[TT])
                yield
                XTt, XTap = sc1, sc1[:, 0:128]
                for kq in range(1, 6):
                    pxt_, pxa = PS()
                    V("pe", lambda e: e.matmul(pxa, XTap, X[:], start=True, stop=True), r=[XTt, X], w=[pxt_])
                    X2 = SB()
                    V("act", lambda e: e.activation(X2[:], pxa, AF.Copy), r=[pxt_], w=[X2])
                    yield
                    if kq < 5:
                        pyt_, pya = PS()
                        V("pe", lambda e: e.matmul(pya, X[:], XTap, start=True, stop=True), r=[X, XTt], w=[pyt_])
                        XT2 = SB()
                        V("dve", lambda e: e.tensor_copy(XT2[:], pya), r=[pyt_], w=[XT2])
                        yield
                    ptt_, pta = PS()
                    V("pe", lambda e: e.matmul(pta, X2[:], TT[:], start=True, stop=True), r=[X2, TT], w=[ptt_])
                    TT2 = SB()
                    V("dve", lambda e: e.tensor_tensor(TT2[:], pta, TT[:], op=ALU.add), r=[ptt_, TT], w=[TT2])
                    yield
                    X = X2; TT = TT2
                    if kq < 5:
                        XTt, XTap = XT2, XT2[:]
                P = Pcur[d]
                put_, pua = PS()
                V("pe", lambda e: e.matmul(pua, sc2[:, 0:128], tv[:], start=True, stop=False), r=[sc2, tv], w=[put_])
                V("pe", lambda e: e.matmul(pua, KRf(0), P[:], start=False, stop=True), r=[kr_t, P], w=[put_])
                U = SB()
                V("act", lambda e: e.activation(U[:], pua, AF.Copy), r=[put_], w=[U])
                yield
                pzt_, pza = PS()
                V("pe", lambda e: e.matmul(pza, TT[:], U[:], start=True, stop=True), r=[TT, U], w=[pzt_])
                Z = SB()
                V("act", lambda e: e.mul(Z[:], pza, -1.0), r=[pzt_], w=[Z])
                yield
                pyt2, pya2 = PS()
                V("pe", lambda e: e.matmul(pya2, P[:], KRf(1), start=True, stop=False), r=[P, kr_t], w=[pyt2])
                V("pe", lambda e: e.matmul(pya2, Z[:], sc1[:, 128:256], start=False, stop=False), r=[Z, sc1], w=[pyt2])
                V("pe", lambda e: e.matmul(pya2, tv[:], sc2[:, 128:256], start=False, stop=True), r=[tv, sc2], w=[pyt2])
                V("dve", lambda e: e.tensor_copy(yd[d][0:64, cs], pya2[0:64, 0:64]), r=[pyt2], w=[yd[d]])
                V("act", lambda e: e.activation(yd[d][64:128, cs], pya2[64:128, 64:128], AF.Copy), r=[pyt2], w=[yd[d]])
                yield
                ppt_, ppa = PS()
                V("pe", lambda e: e.matmul(ppa, ident[:], P[:], start=True, stop=False), r=[ident, P], w=[ppt_])
                V("pe", lambda e: e.matmul(ppa, tak[:, 0, :], Z[:], start=False, stop=False), r=[tak, Z], w=[ppt_])
                V("pe", lambda e: e.matmul(ppa, tak[:, 1, :], tv[:], start=False, stop=True), r=[tak, tv], w=[ppt_])
                Pn = Pst[d][(step + 1) % 3]
                V("dve", lambda e: e.tensor_scalar(Pn[:], ppa, glast[d][:, c:c + 1], None, op0=ALU.mult), r=[ppt_, glast[d]], w=[Pn])
                yield
                Pcur[d] = Pn
                if step % 4 == 3:
                    seq = c // 4
                    pst_, psa = PS()
                    V("pe", lambda e: e.transpose(psa, Pn[:], ident[:]), r=[Pn, ident], w=[pst_])
                    sn = snap[nsnap[0] % 4]; nsnap[0] += 1
                    V("act", lambda e: e.activation(sn[:], psa, AF.Copy), r=[pst_], w=[sn])
                    for hh in range(2):
                        k.dma("sp", ns_d[seq, d, 2 * q + hh], sn[hh * 64:(hh + 1) * 64, hh * 64:(hh + 1) * 64], reads=[sn])
                    if step < NCH - 1:
                        Pk = Pst[d][(step + 2) % 3]
                        V("dve", lambda e: e.tensor_scalar(Pk[:], Pn[:], keepc[:, d, c:c + 1], None, op0=ALU.mult), r=[Pn, keepc], w=[Pk])
                        Pcur[d] = Pk
            for step in range(NCH):
                gens = [unit(step, 0), unit(step, 1)]
                while gens:
                    for g_ in list(gens):
                        try:
                            next(g_)
                        except StopIteration:
                            gens.remove(g_)
            if dbg == "scan1":
                k.barrier()
                for i_, (tt_, ap_) in enumerate(((yd[0], yd[0][:]), (yd[1], yd[1][:]), (KtRt[1], KtRt[1][:, 0, :]), (KtRt[1], KtRt[1][:, 1, :]),
                                                 (AhKh[1], AhKh[1][:, 0, :]), (AhKh[1], AhKh[1][:, 1, :]), (lw, lw[:]), (kap, kap[:]), (g_q, g_q[:]),
                                                 (a_, a_[:]), (cum, cum[:]))):
                    k.dma("sp", dbg_d[i_], ap_, reads=[tt_])
                k.dma("sp", dbg_d[11, :, 0:NCH], glast[1][:], reads=[glast[1]])
                k.barrier()
            V("pool", lambda e: e.tensor_tensor(cum[:], yd[0][:], yd[1][:], op=ALU.add), r=[yd[0], yd[1]], w=[cum])
            V("act", lambda e: e.activation(t0[:], cum[:], AF.Square), r=[cum], w=[t0])
            for half in range(2):
                pm = PB(); p2 = PB()
                V("pe", lambda e: e.matmul(pm[:], blkm[:], cum[:, H(half)], start=True, stop=True), r=[blkm, cum], w=[pm])
                V("pe", lambda e: e.matmul(p2[:], blkm[:], t0[:, H(half)], start=True, stop=True), r=[blkm, t0], w=[p2])
                V("act", lambda e: e.activation(Ep[:, H(half)], pm[:], AF.Copy), r=[pm], w=[Ep])
                V("dve", lambda e: e.tensor_tensor(Em[:, H(half)], Ep[:, H(half)], Ep[:, H(half)], op=ALU.mult), r=[Ep], w=[Em])
                V("dve", lambda e: e.tensor_tensor(Em[:, H(half)], p2[:], Em[:, H(half)], op=ALU.subtract), r=[p2, Em], w=[Em])
            V("dve", lambda e: e.tensor_scalar(Em[:], Em[:], 0.0, GN_EPS, op0=ALU.max, op1=ALU.add), r=[Em], w=[Em])
            V("act", lambda e: e.activation(Em[:], Em[:], AF.Ln), r=[Em], w=[Em])
            V("act", lambda e: e.activation(Em[:], Em[:], AF.Exp, scale=-0.5), r=[Em], w=[Em])
            V("pool", lambda e: e.tensor_tensor(cum[:], cum[:], Ep[:], op=ALU.subtract), r=[cum, Ep], w=[cum])
            V("pool", lambda e: e.tensor_tensor(cum[:], cum[:], Em[:], op=ALU.mult), r=[cum, Em], w=[cum])
            V("dve", lambda e: e.tensor_scalar(cum[:], cum[:], C("gn_g", q), C("gn_b", q), op0=ALU.mult, op1=ALU.add), r=[cum, cols], w=[cum])
            V("pool", lambda e: e.tensor_tensor(cum[:], cum[:], Ex[:], op=ALU.add), r=[cum, Ex], w=[cum])
            V("dve", lambda e: e.tensor_tensor(z_all[:, q, :], cum[:], g_q[:], op=ALU.mult), r=[cum, g_q], w=[zv[q]])
        k.barrier()
        s3.close()
        if dbg == "scan1":
            st.close()
            return
        with contextlib.ExitStack() as s4:
            load_xT(s4)
            wpc = [k.sb([128, NQ, 512], BF16, "wo", s4) for _ in range(2)]
            pj = [k.ps([128, 512], F32, "pjo", s4) for _ in range(2)]
            wv = w_o_d.rearrange("(c p) n -> p c n", p=128)
            npj = 0
            for n_ in range(4):
                wb = wpc[n_ % 2]
                k.dma("pool", wb[:], wv[:, :, n_ * 512:(n_ + 1) * 512], writes=[wb])
                for jj in range(4):
                    qo = n_ * 4 + jj
                    V("pool", lambda e: e.tensor_scalar(XT(qo), XT(qo), ALPHA, None, op0=ALU.mult), r=[xT[qo]], w=[xT[qo]])
                    for half in range(2):
                        pz = pj[npj % 2]; npj += 1
                        for c in range(NQ):
                            V("pe", lambda e: e.matmul(pz[:], wb[:, c, jj * 128:(jj + 1) * 128], z_all[:, c, H(half)],
                                                       start=(c == 0), stop=(c == NQ - 1)), r=[wb, zv[c]], w=[pz])
                        V("dve", lambda e: e.scalar_tensor_tensor(xT_all[:, qo, H(half)], pz[:], ADA(l, 2, qo), xT_all[:, qo, H(half)],
                                                                  op0=ALU.mult, op1=ALU.add), r=[pz, ada, xT[qo]], w=[xT[qo]])
            k.barrier()
        st.close()
        with contextlib.ExitStack() as s5:
            layer_norm(l, 0, s5)
            k.barrier()

    def pool_layer(l=1):
        H = lambda half: slice(half * 512, (half + 1) * 512)
        with contextlib.ExitStack() as s:
            htok = k.sb([128, 8, D], BF16, "htok", s)
            hb = [k.sb([128, NT], BF16, "hb", s) for _ in range(2)]
            hf = [k.sb([128, 512], F32, "hf", s) for _ in range(2)]
            tq = [k.sb([128, 512], F32, "tq", s) for _ in range(2)]
            pT_all = bfA
            pTv = [k.view(pT_all, "pT%d" % q) for q in range(NQ)]
            ic = k.sb([128, 4, NT], F32, "ic", s)
            k.dma("sp", ic[:], poolic_d, writes=[ic])
            psg = k.sb([128, 16], F32, "psg", s)
            V("dve", lambda e: e.tensor_tensor(psg[:], C("pool_scale", 0, 16), ada[:, l, 32:48], op=ALU.mult), r=[cols, ada], w=[psg])
            ptb = [k.ps([128, 8, 128], BF16, "ptb", s) for _ in range(2)]
            pm_ = [k.ps([128, 512], F32, "pm", s) for _ in range(2)]
            py_ = [k.ps([128, 512], F32, "py", s) for _ in range(2)]
            for q in range(NQ):
                h_ = hb[q % 2]; pt = ptb[q % 2]
                V("dve", lambda e: e.tensor_scalar(h_[:], XT(q), OPSC(l, 0, q), ADA(l, 0, q), op0=ALU.mult, op1=ALU.add),
                  r=[xT[q], opsc, ada], w=[h_])
                for tt in range(8):
                    V("pe", lambda e: e.transpose(pt[:, tt, :], h_[:, tt * 128:(tt + 1) * 128], identb[:]), r=[h_, identb], w=[pt])
                V("act", lambda e: e.activation(htok[:, :, q * 128:(q + 1) * 128], pt[:], AF.Copy), r=[pt], w=[htok])
            MT = [k.sb([128, 8, NT], BF16, "MT", s) for _ in range(1)]
            wpl = [k.sb([128, 4, 512], BF16, "wpl", s) for _ in range(2)]
            n1 = 0
            for g in range(4):
                mt = MT[0]; wp = wpl[g % 2]
                k.dma("sp", mt[:], poolMT_d[g].rearrange("(c p) t -> p c t", p=128), writes=[mt])
                k.dma("pool", wp[:], pool_w_d[g].rearrange("(c p) n -> p c n", p=128), writes=[wp])
                for qq in range(4):
                    q = 4 * g + qq
                    for half in range(2):
                        pm = pm_[n1 % 2]; hf_ = hf[n1 % 2]; tq_ = tq[n1 % 2]; n1 += 1
                        for st_ in range(8):
                            V("pe", lambda e: e.matmul(pm[:], htok[:, st_, q * 128:(q + 1) * 128], mt[:, st_, H(half)],
                                                       start=(st_ == 0), stop=(st_ == 7)), r=[htok, mt], w=[pm])
                        V("dve", lambda e: e.tensor_tensor(tq_[:], pm[:], ic[:, g, H(half)], op=ALU.mult), r=[pm, ic], w=[tq_])
                        V("pool", lambda e: e.tensor_scalar(hf_[:], xT_all[:, q, H(half)], OPSC(l, 0, q), ADA(l, 0, q), op0=ALU.mult, op1=ALU.add),
                          r=[xT[q], opsc, ada], w=[hf_])
                        V("dve", lambda e: e.tensor_tensor(pT_all[:, q, H(half)], tq_[:], hf_[:], op=ALU.subtract), r=[tq_, hf_], w=[pTv[q]])
                for dt_ in range(4):
                    qo = 4 * g + dt_
                    V("pool", lambda e: e.tensor_scalar(XT(qo), XT(qo), ALPHA, None, op0=ALU.mult), r=[xT[qo]], w=[xT[qo]])
                    for half in range(2):
                        py = py_[n1 % 2]; n1 += 1
                        for ct in range(4):
                            V("pe", lambda e: e.matmul(py[:], wp[:, ct, dt_ * 128:(dt_ + 1) * 128], pT_all[:, 4 * g + ct, H(half)],
                                                       start=(ct == 0), stop=(ct == 3)), r=[wp, pTv[4 * g + ct]], w=[py])
                        V("dve", lambda e: e.scalar_tensor_tensor(xT_all[:, qo, H(half)], py[:], psg[:, qo:qo + 1], xT_all[:, qo, H(half)],
                                                                  op0=ALU.mult, op1=ALU.add), r=[py, psg, xT[qo]], w=[xT[qo]])
            k.barrier()
        with contextlib.ExitStack() as s5:
            layer_norm(l, 0, s5)
            k.barrier()

    def store_out():
        with contextlib.ExitStack() as s:
            ytok = [k.sb([128, D], F32, "ytok", s) for _ in range(2)]
            tp = [k.ps([128, 4, 128], F32, "tpo", s) for _ in range(2)]
            n = 0
            for tt in range(8):
                yb = ytok[tt % 2]
                for qg in range(4):
                    p = tp[n % 2]; n += 1
                    for j in range(4):
                        q = qg * 4 + j
                        V("pe", lambda e: e.transpose(p[:, j, :], xT_all[:, q, tt * 128:(tt + 1) * 128], ident[:]), r=[xT[q], ident], w=[p])
                    if n % 2:
                        V("act", lambda e: e.activation(yb[:, qg * 512:(qg + 1) * 512], p[:].rearrange("p a b -> p (a b)"), AF.Copy), r=[p], w=[yb])
                    else:
                        V("dve", lambda e: e.tensor_copy(yb[:, qg * 512:(qg + 1) * 512], p[:].rearrange("p a b -> p (a b)")), r=[p], w=[yb])
                k.dma("sp", y_d[tt * 128:(tt + 1) * 128, :], yb[:], reads=[yb])

    def dump_dbg():
        for q in range(NQ):
            k.dma("sp", dbg_d[q], XT(q), reads=[xT[q]])

    stop = dbg
    if stop == "ada":
        with contextlib.ExitStack() as s_:
            load_xT(s_); k.barrier()
        for q in range(6):
            V("dve", lambda e: e.tensor_copy(xT_all[:, q, 0:96], ada[:, 0, :]), r=[ada, xT[q]], w=[xT[q]])
            V("dve", lambda e: e.tensor_copy(xT_all[:, q, 96:192], ada[:, 1, :]), r=[ada, xT[q]], w=[xT[q]])
        dump_dbg()
    elif stop == "mix":
        st = rwkv_layer(0)[0]
        st.close()
    elif stop == "p2":
        st, tw, ta, sgl, negw0, omka = rwkv_layer(0)
        for p_ in range(3):
            for q in range(4):
                k.dma("sp", XT(p_ * 4 + q), rkv_d[p_, q * 5], writes=[xT[p_ * 4 + q]])
        V("dve", lambda e: e.tensor_copy(xT_all[0:96, 12, :], tw[0][:]), r=[tw[0]], w=[xT[12]])
        V("dve", lambda e: e.tensor_copy(xT_all[0:96, 13, :], ta[1][:]), r=[ta[1]], w=[xT[13]])
        V("dve", lambda e: e.tensor_copy(xT_all[:, 14, :], sgl[:, 1, :]), r=[sgl], w=[xT[14]])
        k.barrier()
        st.close()
        dump_dbg()
    elif stop == "scan1":
        st, tw, ta, sgl, negw0, omka = rwkv_layer(0)
        rwkv_scan(0, st, tw, ta, sgl, negw0, omka)
    elif stop == "rwkv":
        st, tw, ta, sgl, negw0, omka = rwkv_layer(0)
        rwkv_scan(0, st, tw, ta, sgl, negw0, omka)
        dump_dbg()
    else:
        st, tw, ta, sgl, negw0, omka = rwkv_layer(0)
        rwkv_scan(0, st, tw, ta, sgl, negw0, omka)
        with contextlib.ExitStack() as s_:
            moe(0, s_)
        with contextlib.ExitStack() as s_:
            layer_norm(0, 1, s_); k.barrier()
        if stop == "l0":
            dump_dbg()
        else:
            pool_layer(1)
            if stop == "pool":
                dump_dbg()
            else:
                with contextlib.ExitStack() as s_:
                    moe(1, s_)
                with contextlib.ExitStack() as s_:
                    layer_norm(1, 1, s_); k.barrier()
    store_out()
    k.finish()
    k.close()
    nc._in_names = in_names
    return nc


def _pool_consts(is_sample):
    MT = np.zeros((4, NT, NT), np.float32)
    ic = np.zeros((4, NT), np.float32)
    for gi, w in enumerate((2, 4, 8, 16)):
        if not is_sample:
            L = 256
            for t in range(NT):
                base = (t // L) * L; tl = t % L
                lo = min(max(tl - w // 2, 0), L); hi = min(max(tl - w // 2 + w, 0), L)
                MT[gi, base + lo:base + hi, t] = 1.0
                ic[gi, t] = 1.0 / (hi - lo)
        else:
            R, W = 16, 64
            for t in range(NT):
                r, c = t // W, t % W
                rlo = min(max(r - w // 2, 0), R); rhi = min(max(r - w // 2 + w, 0), R)
                clo = min(max(c - w // 2, 0), W); chi = min(max(c - w // 2 + w, 0), W)
                for rr in range(rlo, rhi):
                    MT[gi, rr * W + clo:rr * W + chi, t] = 1.0
                ic[gi, t] = 1.0 / ((rhi - rlo) * (chi - clo))
    return MT.astype(ml_dtypes.bfloat16), np.ascontiguousarray(np.broadcast_to(ic[None], (128, 4, NT))).astype(np.float32)


_PROG = {}


def kernel(**inp):
    dbg = inp.pop("_dbg", None)
    inp["_cores"] = inp.pop("_cores", None)
    f32 = lambda a: np.ascontiguousarray(np.asarray(a, np.float32))
    colsa = np.zeros((128, NCOL), np.float32)
    def put(name, v, w=16):
        colsa[:, COLOFF[name]:COLOFF[name] + w] = _colvec(v)
    for l in range(2):
        put("b_ada%d" % l, inp["b_ada"][l], 96)
        for s in range(2):
            put("ln_g%d%d" % (l, s), inp["ln_g"][l, s]); put("ln_b%d%d" % (l, s), inp["ln_b"][l, s])
    for i in range(6):
        put("mp%d" % i, inp["rw_mix_prev"][0, i]); put("mn%d" % i, inp["rw_mix_next"][0, i])
    for d in range(2):
        put("w0%d" % d, inp["rw_w0"][0, d]); put("a0%d" % d, inp["rw_a0"][0, d])
    put("k_k", inp["rw_k_k"][0]); put("k_a", inp["rw_k_a"][0]); put("r_k", np.asarray(inp["rw_r_k"][0]).reshape(-1))
    put("gn_g", inp["rw_gn_g"][0]); put("gn_b", inp["rw_gn_b"][0]); put("pool_scale", inp["pool_scale"][0])
    rbias = np.ascontiguousarray(np.broadcast_to(np.asarray(inp["moe_router_bias"], np.float32)[None], (128, 2, NE)))
    shared = {
        "cols": colsa, "rbias": rbias, "w_ada": f32(inp["w_ada"]),
        "rw_w_r": f32(inp["rw_w_r"][0]), "rw_w_k": f32(inp["rw_w_k"][0]), "rw_w_v": f32(inp["rw_w_v"][0]),
        "rw_w_o": f32(inp["rw_w_o"][0]), "rw_w1": f32(inp["rw_w1"][0]), "rw_w2": f32(inp["rw_w2"][0]),
        "rw_a1": f32(inp["rw_a1"][0]), "rw_a2": f32(inp["rw_a2"][0]), "rw_g1": f32(inp["rw_g1"][0]),
        "rw_g2": f32(inp["rw_g2"][0]), "pool_w": f32(inp["pool_w"][0]), "moe_router": f32(inp["moe_router"]),
        "moe_w_gate": f32(inp["moe_w_gate"]), "moe_w_up": f32(inp["moe_w_up"]), "moe_w_down": f32(inp["moe_w_down"]),
        "moe_ws_gate": f32(inp["moe_ws_gate"]), "moe_ws_up": f32(inp["moe_ws_up"]), "moe_ws_down": f32(inp["moe_ws_down"]),
    }
    pc = {False: _pool_consts(False), True: _pool_consts(True)}
    x_prompt = f32(inp["x_prompt"]); x_sample = f32(inp["x_sample"])
    c = f32(inp["c"]); c_ctx = f32(inp["c_ctx"]); state = f32(inp["state_rwkv"])
    in_maps = []
    for core in range(8):
        samp = core >= 4
        m = dict(shared)
        if not samp:
            m["x"] = np.ascontiguousarray(x_prompt[4 * core:4 * core + 4].reshape(NT, D))
            m["cond"] = _colvec(c_ctx)
            m["s0"] = np.zeros((2, 32, 64, 64), np.float32)
            keep = np.ones((2, NCH), np.float32)
            keep[0, [3, 7, 11]] = 0.0; keep[1, [12, 8, 4]] = 0.0
            tm = np.ones((2, NT), np.float32)
            tm[0, 0::256] = 0.0; tm[1, 255::256] = 0.0
        else:
            b = core - 4
            m["x"] = np.ascontiguousarray(x_sample[b])
            m["cond"] = _colvec(c[b])
            m["s0"] = np.ascontiguousarray(state[b, 0])
            keep = np.ones((2, NCH), np.float32)
            tm = np.ones((2, NT), np.float32)
            tm[0, 0] = 0.0; tm[1, NT - 1] = 0.0
        m["keepc"] = np.ascontiguousarray(np.broadcast_to(keep[None], (128, 2, NCH)))
        m["tmask"] = np.ascontiguousarray(np.broadcast_to(tm[None], (128, 2, NT)))
        m["poolMT"], m["poolic"] = pc[samp]
        in_maps.append(m)
    if dbg not in _PROG:
        _PROG[dbg] = build_program(dbg)
    names = set(_PROG[dbg]._in_names)
    in_maps = [{kk: vv for kk, vv in m.items() if kk in names} for m in in_maps]
    import time as _time
    _t0 = _time.time()
    if dbg is not None and inp.get("_cores") is not None:
        cl = inp["_cores"]
        res = run_bass_kernel_spmd(_PROG[dbg], [in_maps[c_] for c_ in cl], core_ids=list(range(len(cl))))
        return None, {c_: res.results[i]["dbg"] for i, c_ in enumerate(cl)}, {c_: res.results[i]["ns"] for i, c_ in enumerate(cl)}
    res = run_bass_kernel_spmd(_PROG[dbg], in_maps, core_ids=list(range(8)))
    R = res.results
    y_prompt = np.stack([R[cidx]["y"] for cidx in range(4)]).reshape(16, 256, D).astype(np.float32)
    y_sample = np.stack([R[cidx]["y"] for cidx in range(4, 8)]).reshape(4, NT, D).astype(np.float32)
    ns = np.concatenate([R[cidx]["ns"] for cidx in range(4)], axis=0)
    new_state = np.ascontiguousarray(ns[:, None]).astype(np.float32)
    if dbg is not None:
        return (y_prompt, y_sample, new_state), [R[cidx]["dbg"] for cidx in range(8)]
    return (y_prompt, y_sample, new_state)
```
